# Optimizing a Trainium2 kernel written in Bass

```python
import math
import jax, jax.numpy as jnp
from jax import lax
import numpy as np

D_MODEL = 4096
BATCH = 2
SEQ = 8192
DEPTH = 1

HEAD_DIM = 128
N_HEADS_TOTAL = D_MODEL // HEAD_DIM
N_HEADS_SPARSE = N_HEADS_TOTAL // 2
N_KV_SPARSE = N_HEADS_SPARSE // 4
N_HEADS_SB = N_HEADS_TOTAL - N_HEADS_SPARSE
W_SPARSE = N_HEADS_SPARSE * HEAD_DIM
W_SPARSE_KV = N_KV_SPARSE * HEAD_DIM
W_SB = N_HEADS_SB * HEAD_DIM
MIX_WIDTH = W_SPARSE + W_SB
H_IDX = 32
D_IDX = 64
TOPK_MAX = 256
N_BUCKETS = 32
MAX_DISTANCE = 128
D_FF = 4 * D_MODEL
Q_BLOCK = 128
LN_EPS = 1e-5
COL_SIZES = (W_SPARSE, W_SPARSE_KV, W_SPARSE_KV, W_SB, W_SB, W_SB, H_IDX * D_IDX, D_IDX, H_IDX)
IN_COLS = W_SPARSE + 2 * W_SPARSE_KV + 3 * W_SB + H_IDX * D_IDX + D_IDX + H_IDX

kernel_name = "hybrid_dsa_stickbreak_deepnorm_adaln"


def layer_norm(x, g, b):
    xf = x.astype(jnp.float32)
    mu = jnp.mean(xf, axis=-1, keepdims=True)
    xc = xf - mu
    var = jnp.mean(xc * xc, axis=-1, keepdims=True)
    return (xc * lax.rsqrt(var + LN_EPS) * g + b).astype(x.dtype)


def rms_norm(x, g):
    xf = x.astype(jnp.float32)
    ms = jnp.mean(xf * xf, axis=-1, keepdims=True)
    return (xf * lax.rsqrt(ms + LN_EPS) * g).astype(x.dtype)


def t5_bucket(dist):
    n = jnp.maximum(dist, 0)
    max_exact = N_BUCKETS // 2
    nf = jnp.maximum(n, 1).astype(jnp.float32)
    large = max_exact + (jnp.log(nf / max_exact) / math.log(MAX_DISTANCE / max_exact)
                         * (N_BUCKETS - max_exact)).astype(jnp.int32)
    large = jnp.minimum(large, N_BUCKETS - 1)
    return jnp.where(n < max_exact, n, large)


def split_columns(proj):
    parts, start = [], 0
    for size in COL_SIZES:
        parts.append(proj[..., start:start + size])
        start += size
    return parts


def sparse_attention(q, k, v, iq, ik, iw, rel_bias, topk):
    B, S, H, Dh = q.shape
    G = k.shape[2]
    R = H // G
    pos = jnp.arange(S, dtype=jnp.int32)
    b_idx = jnp.arange(B)[:, None, None]

    def block(i):
        t0 = i * Q_BLOCK
        tq = t0 + jnp.arange(Q_BLOCK, dtype=jnp.int32)
        q_blk = lax.dynamic_slice_in_dim(q, t0, Q_BLOCK, axis=1)
        iq_blk = lax.dynamic_slice_in_dim(iq, t0, Q_BLOCK, axis=1)
        iw_blk = lax.dynamic_slice_in_dim(iw, t0, Q_BLOCK, axis=1)
        rel = jax.nn.relu(jnp.einsum('bqhd,bsd->bqhs', iq_blk, ik).astype(jnp.float32)
                          * (D_IDX ** -0.5))
        score = jnp.einsum('bqh,bqhs->bqs', iw_blk.astype(jnp.float32), rel)
        causal = pos[None, :] <= tq[:, None]
        score = jnp.where(causal[None], score, -jnp.inf)
        _, idx = lax.top_k(score, topk)
        valid = idx <= tq[None, :, None]
        k_sel = k[b_idx, idx]
        v_sel = v[b_idx, idx]
        qg = q_blk.reshape(B, Q_BLOCK, G, R, Dh)
        logits = jnp.einsum('bqgrd,bqkgd->bqgrk', qg, k_sel).astype(jnp.float32) * (Dh ** -0.5)
        bias = rel_bias[t5_bucket(tq[None, :, None] - idx)].astype(jnp.float32)
        bias = bias.reshape(B, Q_BLOCK, topk, G, R).transpose(0, 1, 3, 4, 2)
        logits = jnp.where(valid[:, :, None, None, :], logits + bias, -jnp.inf)
        p = jax.nn.softmax(logits, axis=-1).astype(v.dtype)
        o = jnp.einsum('bqgrk,bqkgd->bqgrd', p, v_sel)
        return o.reshape(B, Q_BLOCK, H * Dh)

    out = lax.map(block, jnp.arange(S // Q_BLOCK, dtype=jnp.int32))
    return out.transpose(1, 0, 2, 3).reshape(B, S, H * Dh)


def stick_breaking_attention(q, k, v):
    B, S, H, Dh = q.shape
    pos = jnp.arange(S, dtype=jnp.int32)

    def block(i):
        t0 = i * Q_BLOCK
        tq = t0 + jnp.arange(Q_BLOCK, dtype=jnp.int32)
        q_blk = lax.dynamic_slice_in_dim(q, t0, Q_BLOCK, axis=1)
        z = jnp.einsum('bqhd,bshd->bhqs', q_blk, k).astype(jnp.float32) * (Dh ** -0.5)
        strict = (pos[None, :] < tq[:, None])[None, None]
        log_beta = jax.nn.log_sigmoid(z)
        log_keep = jnp.where(strict, log_beta - z, 0.0)
        later = lax.cumsum(log_keep, axis=3, reverse=True) - log_keep
        a = jnp.where(strict, jnp.exp(log_beta + later), 0.0).astype(v.dtype)
        o = jnp.einsum('bhqs,bshd->bqhd', a, v)
        return o.reshape(B, Q_BLOCK, H * Dh)

    out = lax.map(block, jnp.arange(S // Q_BLOCK, dtype=jnp.int32))
    return out.transpose(1, 0, 2, 3).reshape(B, S, H * Dh)


def setup_inputs(seed: int = 0) -> dict:
    key = jax.random.key(seed)
    ks = jax.random.split(key, 20)
    f32 = jnp.float32
    beta_dn = (8.0 * DEPTH) ** -0.25
    nrm = lambda k, shape, s: jax.random.normal(k, shape, f32) * s
    gain = lambda k, shape: 1.0 + 0.02 * jax.random.normal(k, shape, f32)
    return {
        "x": nrm(ks[0], (BATCH, SEQ, D_MODEL), 1.0),
        "c": nrm(ks[1], (BATCH, D_MODEL), 1.0),
        "in_ln_g": gain(ks[2], (D_MODEL,)),
        "in_ln_b": nrm(ks[3], (D_MODEL,), 0.02),
        "rel_bias": nrm(ks[4], (N_BUCKETS, N_HEADS_SPARSE), 0.5),
        "w_ada": nrm(ks[5], (DEPTH, D_MODEL, 6 * D_MODEL), 0.5 * D_MODEL ** -0.5),
        "b_ada": nrm(ks[6], (DEPTH, 6 * D_MODEL), 0.02),
        "w_in": nrm(ks[7], (DEPTH, D_MODEL, IN_COLS), D_MODEL ** -0.5),
        "idx_kn_g": gain(ks[8], (DEPTH, D_IDX)),
        "idx_kn_b": nrm(ks[9], (DEPTH, D_IDX), 0.02),
        "gn_sparse_g": gain(ks[10], (DEPTH, W_SPARSE)),
        "gn_sb_g": gain(ks[11], (DEPTH, W_SB)),
        "w_out": nrm(ks[12], (DEPTH, MIX_WIDTH, D_MODEL), beta_dn * MIX_WIDTH ** -0.5),
        "ln1_g": gain(ks[13], (DEPTH, D_MODEL)),
        "ln1_b": nrm(ks[14], (DEPTH, D_MODEL), 0.02),
        "w_up": nrm(ks[15], (DEPTH, D_MODEL, D_FF), D_MODEL ** -0.5),
        "w_down": nrm(ks[16], (DEPTH, D_FF, D_MODEL), beta_dn * D_FF ** -0.5),
        "ln2_g": gain(ks[17], (DEPTH, D_MODEL)),
        "ln2_b": nrm(ks[18], (DEPTH, D_MODEL), 0.02),
    }


def reference(x, c, in_ln_g, in_ln_b, rel_bias, w_ada, b_ada, w_in, idx_kn_g, idx_kn_b,
              gn_sparse_g, gn_sb_g, w_out, ln1_g, ln1_b, w_up, w_down, ln2_g, ln2_b):
    B, S, _ = x.shape
    topk = min(TOPK_MAX, S // 4)
    alpha = (2.0 * DEPTH) ** 0.25
    h = layer_norm(x, in_ln_g, in_ln_b)
    cs = jax.nn.silu(c)
    for l in range(DEPTH):
        mod = cs @ w_ada[l] + b_ada[l]
        sh_m, sc_m, g_m, sh_f, sc_f, g_f = [m[:, None, :] for m in jnp.split(mod, 6, axis=-1)]
        u = h * (1.0 + sc_m) + sh_m
        proj = u @ w_in[l]
        aq, ak, av, bq, bk, bv, iq, ik, iw = split_columns(proj)
        aq = aq.reshape(B, S, N_HEADS_SPARSE, HEAD_DIM)
        ak = ak.reshape(B, S, N_KV_SPARSE, HEAD_DIM)
        av = av.reshape(B, S, N_KV_SPARSE, HEAD_DIM)
        bq = bq.reshape(B, S, N_HEADS_SB, HEAD_DIM)
        bk = bk.reshape(B, S, N_HEADS_SB, HEAD_DIM)
        bv = bv.reshape(B, S, N_HEADS_SB, HEAD_DIM)
        iq = iq.reshape(B, S, H_IDX, D_IDX)
        ik = layer_norm(ik, idx_kn_g[l], idx_kn_b[l])
        iw = iw * (H_IDX ** -0.5)
        o_a = sparse_attention(aq, ak, av, iq, ik, iw, rel_bias, topk)
        o_b = stick_breaking_attention(bq, bk, bv)
        mixed = jnp.concatenate([rms_norm(o_a, gn_sparse_g[l]), rms_norm(o_b, gn_sb_g[l])],
                                axis=-1) @ w_out[l]
        h = layer_norm(alpha * h + g_m * mixed, ln1_g[l], ln1_b[l])
        u = h * (1.0 + sc_f) + sh_f
        y = jnp.square(jax.nn.relu(u @ w_up[l])) @ w_down[l]
        h = layer_norm(alpha * h + g_f * y, ln2_g[l], ln2_b[l])
    return h
```

```python
from contextlib import ExitStack
import math
import numpy as np
import concourse.bass as bass
import concourse.mybir as mybir
from concourse.bass_utils import run_bass_kernel_spmd

F32 = mybir.dt.float32
BF16 = mybir.dt.bfloat16
ALU = mybir.AluOpType
AF = mybir.ActivationFunctionType

D = 4096
S = 8192
DFF = 16384
IN_COLS = 11360
EPS = 1e-5
ALPHA = 2.0 ** 0.25
BIG = 30000.0
NBIS = 24
C_AQ, C_AK, C_AV, C_BQ, C_BK, C_BV, C_IQ, C_IK = 0, 2048, 2560, 3072, 5120, 7168, 9216, 11264

ENGS = ("pe", "act", "dve", "pool", "sp")
SEM_EPOCH = 30000
NDMA = 8


class Buf:
    __slots__ = ("w", "r")

    def __init__(self):
        self.w = {}
        self.r = {}


def bufs(n):
    return [Buf() for _ in range(n)]


class KB:
    def __init__(self, nc):
        self.nc = nc
        self.es = ExitStack()
        self.sems = []
        self.prog = {e: [] for e in ENGS}
        self.cur = {}
        self.cnt = {}
        self.waited = {e: {} for e in ENGS}
        for e in ENGS:
            self.cur[e] = self._newsem()
            self.cnt[e] = 0
        self.dpool = {q: [self._newsem() for _ in range(NDMA)] for q in ("sp", "pool")}
        self.dval = {q: [0] * NDMA for q in ("sp", "pool")}
        self.dnext = {q: 0 for q in ("sp", "pool")}
        self.ninst = 0

    def _newsem(self):
        s = self.es.enter_context(self.nc.semaphore())
        self.sems.append(s)
        return len(self.sems) - 1

    def _wait(self, eng, needs):
        wd = self.waited[eng]
        for s, v in needs.items():
            if wd.get(s, 0) >= v:
                continue
            wd[s] = v
            sem = self.sems[s]
            self.prog[eng].append(lambda e, sem=sem, v=v: e.wait_ge(sem, v))

    def _needs(self, eng, r, w):
        needs = {}
        own = self.cur[eng]
        for b in r:
            for s, v in b.w.items():
                if needs.get(s, 0) < v:
                    needs[s] = v
        for b in w:
            for s, v in b.w.items():
                if s != own and needs.get(s, 0) < v:
                    needs[s] = v
            for s, v in b.r.items():
                if s != own and needs.get(s, 0) < v:
                    needs[s] = v
        return needs

    def _commit(self, s, v, r, w):
        for b in w:
            b.w = {s: v}
            b.r = {}
        for b in r:
            if b.r.get(s, 0) < v:
                b.r[s] = v

    def op(self, eng, fn, r=(), w=()):
        if self.cnt[eng] >= SEM_EPOCH:
            self.cur[eng] = self._newsem()
            self.cnt[eng] = 0
        self._wait(eng, self._needs(eng, r, w))
        s = self.cur[eng]
        self.cnt[eng] += 1
        v = self.cnt[eng]
        sem = self.sems[s]
        self.prog[eng].append(lambda e, fn=fn, sem=sem: fn(e).then_inc(sem, 1))
        self._commit(s, v, r, w)
        self.ninst += 1

    def dma(self, q, out, in_, r=(), w=()):
        i = self.dnext[q]
        self.dnext[q] = (i + 1) % NDMA
        s = self.dpool[q][i]
        needs = self._needs(q, r, w)
        if self.dval[q][i] > 0:
            needs[s] = max(needs.get(s, 0), self.dval[q][i])
        self._wait(q, needs)
        self.dval[q][i] += 16
        v = self.dval[q][i]
        sem = self.sems[s]
        self.prog[q].append(lambda e, out=out, in_=in_, sem=sem: e.dma_start(out=out, in_=in_).then_inc(sem, 16))
        self._commit(s, v, r, w)
        self.ninst += 1

    def barrier(self):
        latest = {}
        for e in ENGS:
            if self.cnt[e] > 0:
                latest[self.cur[e]] = self.cnt[e]
        for q in self.dpool:
            for i, s in enumerate(self.dpool[q]):
                if self.dval[q][i] > 0:
                    latest[s] = self.dval[q][i]
        for e in ENGS:
            self._wait(e, dict(latest))

    def finish(self):
        prog = self.prog
        with self.nc.Block() as block:
            @block.sync
            def _(e):
                for f in prog["sp"]:
                    f(e)

            @block.tensor
            def _(e):
                for f in prog["pe"]:
                    f(e)

            @block.scalar
            def _(e):
                for f in prog["act"]:
                    f(e)

            @block.vector
            def _(e):
                for f in prog["dve"]:
                    f(e)

            @block.gpsimd
            def _(e):
                for f in prog["pool"]:
                    f(e)
        self.es.close()

    def mm(self, out, lhsT, rhs, start, stop, r, w):
        self.op("pe", lambda e: e.matmul(out, lhsT=lhsT, rhs=rhs, start=start, stop=stop), r, w)

    def act(self, out, in_, func, r, w, scale=1.0, bias=0.0):
        self.op("act", lambda e: e.activation(out=out, in_=in_, func=func, bias=bias, scale=scale), r, w)

    def ts(self, eng, out, in0, s1, s2, op0, op1, r, w, accum=None):
        if op1 is None:
            self.op(eng, lambda e: e.tensor_scalar(out=out, in0=in0, scalar1=s1, scalar2=None, op0=op0), r, w)
        elif accum is None:
            self.op(eng, lambda e: e.tensor_scalar(out=out, in0=in0, scalar1=s1, scalar2=s2, op0=op0, op1=op1), r, w)
        else:
            self.op(eng, lambda e: e.tensor_scalar(out=out, in0=in0, scalar1=s1, scalar2=s2, op0=op0, op1=op1,
                                                   accum_out=accum), r, w)

    def tt(self, eng, out, in0, in1, op, r, w):
        self.op(eng, lambda e: e.tensor_tensor(out=out, in0=in0, in1=in1, op=op), r, w)

    def stt(self, eng, out, in0, scalar, in1, op0, op1, r, w):
        self.op(eng, lambda e: e.scalar_tensor_tensor(out=out, in0=in0, scalar=scalar, in1=in1, op0=op0, op1=op1), r, w)

    def cp(self, eng, out, in_, r, w):
        if eng == "act":
            self.op("act", lambda e: e.activation(out=out, in_=in_, func=AF.Copy), r, w)
        else:
            self.op(eng, lambda e: e.tensor_copy(out=out, in_=in_), r, w)


class Ring:
    def __init__(self, tiles):
        self.t = tiles
        self.b = bufs(len(tiles))
        self.i = 0

    def next(self):
        i = self.i
        self.i = (i + 1) % len(self.t)
        return self.t[i], self.b[i]


def build(stop_after="G", debug=False):
    nc = bass.Bass("TRN2", target_bir_lowering=False)
    K = KB(nc)
    names = [0]

    def din(name, shape, dt=F32):
        return nc.dram_tensor(name, list(shape), dt, kind="ExternalInput").ap()

    def dscr(name, shape, dt):
        kind = "ExternalOutput" if (debug and name in debug) else "Internal"
        return nc.dram_tensor(name, list(shape), dt, kind=kind).ap()

    def sb(stack, shape, dt):
        names[0] += 1
        return stack.enter_context(nc.sbuf_tensor(f"sb{names[0]}", list(shape), dt))

    xpad = din("xpad", [S, D])
    kvalT_d = din("kvalT", [128, 64])
    kvrow_d = din("kvrow", [1, S])
    cT_d = din("cT", [128, 32])
    badaT_d = din("badaT", [128, 192])
    prmT_d = din("prmT", [128, 5, 32])
    bc_d = din("bc", [6, 128, D])
    ikgb_d = din("ikgb", [128, 128])
    rb31_d = din("rb31", [128, 16])
    Bt_d = din("Bt", [16, 6, 128, 512])
    identf_d = din("identf", [128, 128])
    tri_d = din("tri", [128, 128])
    cneg_d = din("cneg", [4, 128, 512])
    cm_d = din("cm", [128, 4, 512])
    w_ada = din("w_ada", [D, 6 * D])
    w_in = din("w_in", [D, IN_COLS])
    w_out = din("w_out", [D, D])
    w_up = din("w_up", [D, DFF])
    w_down = din("w_down", [DFF, D])
    out_d = nc.dram_tensor("out", [2048, D], F32, kind="ExternalOutput").ap()

    kTb_d = dscr("kTb", [16, 128, S], BF16)
    kTa_d = dscr("kTa", [4, 128, S], BF16)
    vb_d = dscr("vb", [S, 2048], BF16)
    va_d = dscr("va", [S, 512], BF16)
    ikT_d = dscr("ikT", [64, S], BF16)
    iw_d = dscr("iw", [S, 32], F32)
    qTa_d = dscr("qTa", [16, 128, 2048], BF16)
    qTb_d = dscr("qTb", [16, 128, 2048], BF16)
    iqT_d = dscr("iqT", [16, 128, 2048], BF16)
    maskT_d = dscr("maskT", [4, 64, 128, 512], BF16)
    oT_d = dscr("oT", [32, 128, 2048], F32)
    ssq_d = dscr("ssq", [32, 2048], F32)
    mixT_d = dscr("mixT", [32, 128, 2048], F32)
    h1_d = dscr("h1", [2048, D], F32)
    u2T_d = dscr("u2T", [128, 32, 2048], BF16)
    yT_d = dscr("yT", [4, 32, 128, 2048], F32)
    modT_dbg = dscr("modT", [128, 192], F32) if (debug and "modT" in debug) else None

    G = ExitStack()
    psb = [G.enter_context(nc.psum_tensor(f"psb{i}", [128, 1024], F32)) for i in range(4)]
    ps = []
    for j_ in range(4):
        ps += [psb[j_][:, 0:512], psb[j_][:, 512:1024]]
    Bps = bufs(8)

    identf = sb(G, [128, 128], F32)
    identb = sb(G, [128, 128], BF16)
    onesb = sb(G, [128, 128], BF16)
    onesf = sb(G, [128, 128], F32)
    modT = sb(G, [128, 192], F32)
    prmT = sb(G, [128, 5, 32], F32)
    AB = sb(G, [128, 4, 32], F32)
    kvalT = sb(G, [128, 64], F32)
    Bconst = Buf()
    Bmod = Buf()

    K.dma("sp", identf[:], identf_d[:, :], w=[Bconst])
    K.dma("sp", prmT[:], prmT_d[:, :, :], w=[Bconst])
    K.dma("sp", kvalT[:], kvalT_d[:, :], w=[Bconst])
    K.cp("dve", identb[:], identf[:], [Bconst], [Bconst])
    K.op("dve", lambda e: e.memset(onesb[:], 1.0), w=[Bconst])
    K.op("dve", lambda e: e.memset(onesf[:], 1.0), w=[Bconst])

    with ExitStack() as ph:
        cT = sb(ph, [128, 32], F32)
        cs = sb(ph, [128, 32], F32)
        badaT = sb(ph, [128, 192], F32)
        row = [sb(ph, [1, 2048], F32) for _ in range(2)]
        Brow = bufs(2)
        wa = Ring([sb(ph, [128, 2048], F32) for _ in range(4)])
        Bc = Buf()
        K.dma("sp", cT[:], cT_d[:, :], w=[Bc])
        K.dma("sp", badaT[:], badaT_d[:, :], w=[Bc])
        K.act(cs[:], cT[:], AF.Silu, [Bc], [Bc])
        psM, BpsM = ps[4], Bps[4]
        for grp in range(12):
            for kc in range(32):
                wt, Bw = wa.next()
                K.dma("sp", wt[:], w_ada[kc * 128:(kc + 1) * 128, grp * 2048:(grp + 1) * 2048], w=[Bw])
                for n in range(4):
                    K.mm(ps[n][0:1, :], cs[:, kc:kc + 1], wt[:, n * 512:(n + 1) * 512], kc == 0, kc == 31,
                         [Bc, Bw], [Bps[n]])
            rw, Br = row[grp % 2], Brow[grp % 2]
            for n in range(4):
                K.cp("act" if n % 2 == 0 else "dve", rw[0:1, n * 512:(n + 1) * 512], ps[n][0:1, :], [Bps[n]], [Br])
            for jj in range(16):
                col = grp * 16 + jj
                K.mm(psM[:, col:col + 1], rw[0:1, jj * 128:(jj + 1) * 128], onesf[0:1, 0:1], True, True,
                     [Br, Bconst], [BpsM])
        K.tt("dve", modT[:], psM[:, 0:192], badaT[:], ALU.add, [BpsM, Bc], [Bmod])
        K.stt("dve", AB[:, 0, :], modT[:, 32:64], 1.0, prmT[:, 0, :], ALU.add, ALU.mult, [Bmod, Bconst], [Bmod])
        K.stt("dve", AB[:, 1, :], modT[:, 32:64], 1.0, prmT[:, 1, :], ALU.add, ALU.mult, [Bmod, Bconst], [Bmod])
        K.tt("dve", AB[:, 1, :], AB[:, 1, :], modT[:, 0:32], ALU.add, [Bmod], [Bmod])
        K.stt("dve", AB[:, 2, :], modT[:, 128:160], 1.0, prmT[:, 2, :], ALU.add, ALU.mult, [Bmod, Bconst], [Bmod])
        K.stt("dve", AB[:, 3, :], modT[:, 128:160], 1.0, prmT[:, 3, :], ALU.add, ALU.mult, [Bmod, Bconst], [Bmod])
        K.tt("dve", AB[:, 3, :], AB[:, 3, :], modT[:, 96:128], ALU.add, [Bmod], [Bmod])
        if modT_dbg is not None:
            K.dma("sp", modT_dbg[:, :], modT[:], r=[Bmod])
        K.barrier()

    def ln_stats(xt, Bx, stats, st, Bst):
        for i in range(8):
            K.op("dve", lambda e, i=i: e.bn_stats(out=stats[:, i * 6:(i + 1) * 6], in_=xt[:, i * 512:(i + 1) * 512]),
                 [Bx], [Bst])
        K.op("dve", lambda e: e.bn_aggr(out=st[:, 0:2], in_=stats[:, 0:48]), [Bst], [Bst])
        K.act(st[:, 2:3], st[:, 1:2], AF.Sqrt, [Bst], [Bst], scale=1.0, bias=EPS)
        K.op("dve", lambda e: e.reciprocal(out=st[:, 4:5], in_=st[:, 2:3]), [Bst], [Bst])
        K.stt("dve", st[:, 5:6], st[:, 0:1], -1.0, st[:, 4:5], ALU.mult, ALU.mult, [Bst], [Bst])

    def to_featmajor(xh, Bxh, Acol, Bcol, dst_fn, Bdst_fn, psbanks):
        for k4 in range(8):
            pb = psbanks[k4 % len(psbanks)]
            for i in range(4):
                kc = k4 * 4 + i
                K.mm(ps[pb][:, i * 128:(i + 1) * 128], xh[:, kc * 128:(kc + 1) * 128], identb[:], True, True,
                     [Bxh, Bconst], [Bps[pb]])
            for i in range(4):
                kc = k4 * 4 + i
                if k4 % 2 == 0:
                    K.act(dst_fn(kc), ps[pb][:, i * 128:(i + 1) * 128], AF.Identity, [Bps[pb], Bmod], [Bdst_fn(kc)],
                          scale=Acol[:, kc:kc + 1], bias=Bcol[:, kc:kc + 1])
                else:
                    K.ts("dve", dst_fn(kc), ps[pb][:, i * 128:(i + 1) * 128], Acol[:, kc:kc + 1], Bcol[:, kc:kc + 1],
                         ALU.mult, ALU.add, [Bps[pb], Bmod], [Bdst_fn(kc)])

    class Proj:
        def __init__(self, stack):
            self.wt = [sb(stack, [128, 32, 128], BF16) for _ in range(3)]
            self.Bwt = [bufs(4) for _ in range(3)]
            self.pair = 0
            self.pending = None

        def run(self, W3, tiles, mov, movbufs, halves, npairs=3, hooks=None):
            n = len(tiles)

            def load(i):
                c0, width, _ = tiles[i]
                sl = i % 3
                for q in range(4):
                    K.dma("pool", self.wt[sl][:, 8 * q:8 * q + 8, 0:width], W3[:, 8 * q:8 * q + 8, c0:c0 + width],
                          w=[self.Bwt[sl][q]])

            load(0)
            if n > 1:
                load(1)
            for i in range(n):
                c0, width, epi = tiles[i]
                sl = i % 3
                if i + 2 < n:
                    load(i + 2)
                p = self.pair
                self.pair = (p + 1) % npairs
                pss = {h: ps[2 * p + hi] for hi, h in enumerate(halves)}
                Bpss = {h: Bps[2 * p + hi] for hi, h in enumerate(halves)}
                for kc in range(32):
                    for h in halves:
                        K.mm(pss[h][0:width, :], self.wt[sl][:, kc, 0:width], mov[:, kc, h * 512:(h + 1) * 512],
                             kc == 0, kc == 31, [self.Bwt[sl][kc // 8]] + movbufs(kc, h), [Bpss[h]])
                if hooks and i in hooks:
                    hooks[i]()
                if self.pending is not None:
                    self.pending()
                    self.pending = None
                self.pending = epi(pss, Bpss, i)
            if self.pending is not None:
                self.pending()
                self.pending = None

    def w3(w, r0=0):
        return w[r0:r0 + D, :].rearrange("(kc p) c -> p kc c", p=128)

    if stop_after >= "B":
        with ExitStack() as ph:
            uTs = [sb(ph, [128, 32, 1024], BF16) for _ in range(2)]
            BuTs = [[bufs(8) for _ in range(8)] for _ in range(2)]
            xr = Ring([sb(ph, [128, D], F32) for _ in range(1)])
            xhr = Ring([sb(ph, [128, D], BF16) for _ in range(1)])
            stats = sb(ph, [128, 48], F32)
            st = sb(ph, [128, 8], F32)
            Bst = Buf()
            stg = Ring([sb(ph, [128, 1024], BF16) for _ in range(3)])
            vtok = Ring([sb(ph, [128, 8, 128], BF16) for _ in range(2)])
            ikst = sb(ph, [128, 1024], F32)
            Bikst = Buf()
            ikt = sb(ph, [128, 96], F32)
            ikn = sb(ph, [128, 64], F32)
            iknb = sb(ph, [128, 64], BF16)
            ikstats = sb(ph, [128, 6], F32)
            ist = sb(ph, [128, 8], F32)
            Bik = Buf()
            iwt = sb(ph, [128, 8, 32], F32)
            Biw = Buf()
            ikTs = sb(ph, [64, 1024], BF16)
            BikTs = Buf()
            ikgb = sb(ph, [128, 128], F32)
            K.dma("sp", ikgb[:], ikgb_d[:, :], w=[Bconst])
            pj = Proj(ph)
            W3in = w3(w_in)
            xrot = [6, 7]
            xri = [0]

            def nextx():
                b = xrot[xri[0] % 2]
                xri[0] += 1
                return b

            b1state = {}

            def emit_B1a(g, tb):
                xt, Bx = xr.next()
                xh, Bxh = xhr.next()
                r0 = g * 1024 + tb * 128
                K.dma("sp", xt[:], xpad[r0:r0 + 128, :], w=[Bx])
                ln_stats(xt, Bx, stats, st, Bst)
                K.act(xh[:], xt[:], AF.Identity, [Bx, Bst], [Bxh], scale=st[:, 4:5], bias=st[:, 5:6])
                b1state[(g, tb)] = (xh, Bxh)

            def emit_B1b(g, tb):
                uT, BuT = uTs[g % 2], BuTs[g % 2]
                xh, Bxh = b1state.pop((g, tb))
                to_featmajor(xh, Bxh, AB[:, 0, :], AB[:, 1, :],
                             lambda kc, tb=tb: uT[:, kc, tb * 128:(tb + 1) * 128],
                             lambda kc, tb=tb: BuT[tb][kc // 4], [6, 7])

            def emit_B1(g):
                for tb in range(8):
                    emit_B1a(g, tb)
                    emit_B1b(g, tb)

            emit_B1(0)
            for g in range(8):
                uT, BuT = uTs[g % 2], BuTs[g % 2]

                def movbufs(kc, h, BuT=BuT):
                    return [BuT[tb][kc // 4] for tb in range(h * 4, h * 4 + 4)]

                def epi_kT(dst, scale, halves, dcol):
                    def epi(pss, Bpss, idx):
                        s_, Bs = stg.next()
                        for hi, h in enumerate(halves):
                            if (idx + hi) % 2 == 0:
                                K.act(s_[:, hi * 512:(hi + 1) * 512], pss[h][:, :], AF.Copy, [Bpss[h]], [Bs], scale=scale)
                            else:
                                K.ts("dve", s_[:, hi * 512:(hi + 1) * 512], pss[h][:, :], scale, None, ALU.mult, None,
                                     [Bpss[h]], [Bs])
                        n = 512 * len(halves)
                        K.dma("sp", dst[:, dcol:dcol + n], s_[:, 0:n], r=[Bs])
                        return None
                    return epi

                def epi_v(dst3, ccol, g=g):
                    def epi(pss, Bpss, idx):
                        s_, Bs = stg.next()
                        for h in (0, 1):
                            if (idx + h) % 2 == 0:
                                K.act(s_[:, h * 512:(h + 1) * 512], pss[h][:, :], AF.Copy, [Bpss[h]], [Bs])
                            else:
                                K.cp("dve", s_[:, h * 512:(h + 1) * 512], pss[h][:, :], [Bpss[h]], [Bs])

                        def deferred():
                            vt, Bv = vtok.next()
                            for hb in range(2):
                                pb = nextx()
                                for i in range(4):
                                    tb = hb * 4 + i
                                    K.mm(ps[pb][:, i * 128:(i + 1) * 128], s_[:, tb * 128:(tb + 1) * 128], identb[:],
                                         True, True, [Bs, Bconst], [Bps[pb]])
                                for i in range(4):
                                    tb = hb * 4 + i
                                    blk = g * 8 + tb
                                    K.ts("dve", vt[:, tb, :], ps[pb][:, i * 128:(i + 1) * 128], kvalT[:, blk:blk + 1], None,
                                         ALU.mult, None, [Bps[pb], Bconst], [Bv])
                            K.dma("sp", dst3[:, g * 8:(g + 1) * 8, ccol:ccol + 128], vt[:], r=[Bv])
                        return deferred
                    return epi

                def epi_ik(pss, Bpss, idx, g=g):
                    for h in (0, 1):
                        K.cp("act", ikst[0:96, h * 512:(h + 1) * 512], pss[h][0:96, :], [Bpss[h]], [Bikst])

                    def deferred():
                        for tb in range(8):
                            pb = nextx()
                            K.mm(ps[pb][:, 0:96], ikst[0:96, tb * 128:(tb + 1) * 128], identf[0:96, 0:96], True, True,
                                 [Bikst, Bconst], [Bps[pb]])
                            K.cp("dve", ikt[:], ps[pb][:, 0:96], [Bps[pb]], [Bik])
                            K.op("dve", lambda e: e.bn_stats(out=ikstats[:], in_=ikt[:, 0:64]), [Bik], [Bik])
                            K.op("dve", lambda e: e.bn_aggr(out=ist[:, 0:2], in_=ikstats[:]), [Bik], [Bik])
                            K.act(ist[:, 2:3], ist[:, 1:2], AF.Sqrt, [Bik], [Bik], scale=1.0, bias=EPS)
                            K.op("dve", lambda e: e.reciprocal(out=ist[:, 4:5], in_=ist[:, 2:3]), [Bik], [Bik])
                            K.stt("dve", ist[:, 5:6], ist[:, 0:1], -1.0, ist[:, 4:5], ALU.mult, ALU.mult, [Bik], [Bik])
                            K.ts("dve", ikn[:], ikt[:, 0:64], ist[:, 4:5], ist[:, 5:6], ALU.mult, ALU.add, [Bik], [Bik])
                            K.tt("dve", ikn[:], ikn[:], ikgb[:, 0:64], ALU.mult, [Bik, Bconst], [Bik])
                            K.tt("dve", iknb[:], ikn[:], ikgb[:, 64:128], ALU.add, [Bik, Bconst], [Bik])
                            K.ts("dve", iwt[:, tb, :], ikt[:, 64:96], 32.0 ** -0.5, None, ALU.mult, None, [Bik], [Biw])
                            pb2 = nextx()
                            K.mm(ps[pb2][0:64, 0:128], iknb[:], identb[:], True, True, [Bik, Bconst], [Bps[pb2]])
                            K.cp("act", ikTs[:, tb * 128:(tb + 1) * 128], ps[pb2][0:64, 0:128], [Bps[pb2]], [BikTs])
                        K.dma("sp", ikT_d[:, g * 1024:(g + 1) * 1024], ikTs[:], r=[BikTs])
                        K.dma("sp", iw_d.rearrange("(kb p) c -> p kb c", p=128)[:, g * 8:(g + 1) * 8, :], iwt[:], r=[Biw])
                    return deferred

                tiles = []
                for h in range(16):
                    tiles.append((C_BK + h * 128, 128, epi_kT(kTb_d[h], 1.0, (0, 1), g * 1024)))
                for h in range(4):
                    tiles.append((C_AK + h * 128, 128, epi_kT(kTa_d[h], 1.0, (0, 1), g * 1024)))
                vb3 = vb_d.rearrange("(kb p) c -> p kb c", p=128)
                va3 = va_d.rearrange("(kb p) c -> p kb c", p=128)
                for h in range(16):
                    tiles.append((C_BV + h * 128, 128, epi_v(vb3, h * 128)))
                for h in range(4):
                    tiles.append((C_AV + h * 128, 128, epi_v(va3, h * 128)))
                tiles.append((C_IK, 96, epi_ik))
                hooks = {}
                if g + 1 < 8:
                    for tb in range(8):
                        hooks[4 * tb + 2] = (lambda g=g, tb=tb: emit_B1a(g + 1, tb))
                        hooks[4 * tb + 5] = (lambda g=g, tb=tb: emit_B1b(g + 1, tb))
                pj.run(W3in, tiles, uT, movbufs, (0, 1), hooks=hooks)
                if g % 2 == 1:
                    k = g // 2
                    qt = []
                    for h in range(16):
                        qt.append((C_AQ + h * 128, 128, epi_kT(qTa_d[h], 128.0 ** -0.5, (1,), k * 512)))
                    for h in range(16):
                        qt.append((C_BQ + h * 128, 128, epi_kT(qTb_d[h], 128.0 ** -0.5, (1,), k * 512)))
                    for h in range(16):
                        qt.append((C_IQ + h * 128, 128, epi_kT(iqT_d[h], 0.125, (1,), k * 512)))
                    pj.run(W3in, qt, uT, movbufs, (1,))
            K.barrier()

    if stop_after >= "C":
        with ExitStack() as ph:
            ikT2 = sb(ph, [128, S], BF16)
            kvrow = sb(ph, [1, S], F32)
            cneg = sb(ph, [128, 4, 512], BF16)
            cnegf = sb(ph, [128, 512], F32)
            Bk = Buf()
            K.dma("sp", ikT2[0:64, :], ikT_d[:, :], w=[Bk])
            K.dma("sp", ikT2[64:128, :], ikT_d[:, :], w=[Bk])
            K.dma("sp", kvrow[:], kvrow_d[:, :], w=[Bk])
            K.ts("dve", kvrow[:], kvrow[:], -1.0, BIG, ALU.add, ALU.mult, [Bk], [Bk])
            for r_ in range(4):
                K.dma("sp", cnegf[:], cneg_d[r_], w=[Bk])
                K.cp("dve", cneg[:, r_, :], cnegf[:], [Bk], [Bk])
            score = [sb(ph, [128, S], F32) for _ in range(2)]
            Bscore = bufs(2)
            maskb = sb(ph, [128, S], BF16)
            junk = maskb
            Bmask = Buf()
            diags = [sb(ph, [128, 32, 128], BF16) for _ in range(2)]
            Bdiags = bufs(2)
            iqs = Ring([sb(ph, [128, 16, 128], BF16) for _ in range(2)])
            iws = Ring([sb(ph, [128, 32], F32) for _ in range(2)])
            rb = Ring([sb(ph, [128, 1024], BF16) for _ in range(3)])
            mts = Ring([sb(ph, [128, 4, 128], BF16) for _ in range(3)])
            lo = sb(ph, [128, 4], F32)
            Blo = Buf()
            zi = [0]
            iw3 = iw_d.rearrange("(kb p) c -> p kb c", p=128)
            pend_tr = [None]

            def emit_transposes(k, r_, nk):
                for kb4 in range(nk):
                    pb = 6 + (kb4 % 2)
                    for i in range(4):
                        kb = kb4 * 4 + i
                        K.mm(ps[pb][:, i * 128:(i + 1) * 128], maskb[:, kb * 128:(kb + 1) * 128], identb[:], True, True,
                             [Bmask, Bconst], [Bps[pb]])
                    mt, Bmt = mts.next()
                    K.cp("dve", mt[:].rearrange("p a b -> p (a b)"), ps[pb][:, :], [Bps[pb]], [Bmt])
                    K.dma("sp", maskT_d[k, kb4 * 4:(kb4 + 1) * 4, :, r_ * 128:(r_ + 1) * 128].rearrange("a p c -> p a c"),
                          mt[:], r=[Bmt])

            qstate = {}

            def prefetch_q(qb):
                if qb >= 16 or qb in qstate:
                    return
                k, r_ = qb // 4, qb % 4
                pblk = (4 * k + 3) * 4 + r_
                iq_, Biq = iqs.next()
                iw_, Biw_ = iws.next()
                K.dma("sp", iq_[:], iqT_d[:, :, qb * 128:(qb + 1) * 128].rearrange("t p c -> p t c"), w=[Biq])
                K.dma("sp", iw_[:], iw3[:, pblk, :], w=[Biw_])
                diag, Bdiag = diags[qb % 2], Bdiags[qb % 2]
                for h in range(32):
                    K.ts("pool", diag[:, h, :], identb[:], iw_[:, h:h + 1], None, ALU.mult, None,
                         [Bconst, Biw_], [Bdiag])
                qstate[qb] = (iq_, Biq, diag, Bdiag)

            prefetch_q(0)
            for qb in range(16):
                k, r_ = qb // 4, qb % 4
                nk = 4 * (k + 1)
                nkeys = nk * 512
                iq_, Biq, diag, Bdiag = qstate[qb]
                prefetch_q(qb + 1)
                sc, Bsc = score[qb % 2], Bscore[qb % 2]
                for kc in range(nk):
                    sp_ = 4 + (kc % 2)
                    pend = None
                    for hp2 in range(16):
                        zj = zi[0] % 2
                        zi[0] += 1
                        for sub in (0, 1):
                            hp = 64 * sub
                            K.mm(ps[2 * zj + sub][:, :], iq_[hp:hp + 64, hp2, :], ikT2[hp:hp + 64, kc * 512:(kc + 1) * 512], True, True,
                                 [Biq, Bk], [Bps[2 * zj + sub]])
                        if pend is not None:
                            pend()
                        rt, Br = rb.next()
                        K.act(rt[:], psb[zj][:, :], AF.Relu, [Bps[2 * zj], Bps[2 * zj + 1]], [Br])

                        def pend(hp2=hp2, rt=rt, Br=Br, sp_=sp_, diag=diag, Bdiag=Bdiag):
                            for sub in (0, 1):
                                h = 2 * hp2 + sub
                                K.mm(ps[sp_][:, :], diag[:, h, :], rt[:, sub * 512:(sub + 1) * 512], h == 0, False, [Bdiag, Br],
                                     [Bps[sp_]])
                    pend()
                    last = (kc == nk - 1)
                    K.mm(ps[sp_][:, :], onesf[0:1, :], kvrow[0:1, kc * 512:(kc + 1) * 512], False, not last, [Bconst, Bk],
                         [Bps[sp_]])
                    if last:
                        K.mm(ps[sp_][:, :], identb[:], cneg[:, r_, :], False, True, [Bconst, Bk], [Bps[sp_]])
                    K.cp("act", sc[:, kc * 512:(kc + 1) * 512], ps[sp_][:, :], [Bps[sp_]], [Bsc])
                if pend_tr[0] is not None:
                    emit_transposes(*pend_tr[0])
                K.op("dve", lambda e: e.memset(lo[:, 0:1], -64.0), w=[Blo])
                step = 64.0
                for it in range(NBIS):
                    K.ts("dve", lo[:, 1:2], lo[:, 0:1], step, None, ALU.add, None, [Blo], [Blo])
                    K.ts("dve", junk[:, 0:nkeys], sc[:, 0:nkeys], lo[:, 1:2], 0.0, ALU.is_ge, ALU.add, [Bsc, Blo], [Blo, Bmask],
                         accum=lo[:, 2:3])
                    K.ts("dve", lo[:, 3:4], lo[:, 2:3], 255.5, step, ALU.is_ge, ALU.mult, [Blo], [Blo])
                    K.tt("dve", lo[:, 0:1], lo[:, 0:1], lo[:, 3:4], ALU.add, [Blo], [Blo])
                    step *= 0.5
                K.ts("dve", maskb[:, 0:nkeys], sc[:, 0:nkeys], lo[:, 0:1], None, ALU.is_ge, None, [Bsc, Blo], [Bmask])
                pend_tr[0] = (k, r_, nk)
            emit_transposes(*pend_tr[0])
            K.barrier()

    def run_pipeline(stages, items, reverse=False):
        n = len(items)
        ns = len(stages)
        for step in range(n + ns - 1):
            order = list(enumerate(stages))
            if reverse:
                order = order[::-1]
            for si, f in order:
                i = step - si
                if f is not None and 0 <= i < n:
                    f(items[i])

    if stop_after >= "D":
        with ExitStack() as ph:
            kT = Ring([sb(ph, [128, S], BF16) for _ in range(2)])
            vv = Ring([sb(ph, [128, 64, 128], BF16) for _ in range(2)])
            qq = Ring([sb(ph, [128, 4, 512], BF16) for _ in range(2)])
            mk = Ring([sb(ph, [128, 4, 512], BF16) for _ in range(6)])
            btr = Ring([sb(ph, [128, 2, 512], F32) for _ in range(6)])
            tmpr = Ring([sb(ph, [128, 1024], F32) for _ in range(3)])
            er = Ring([sb(ph, [128, 1024], BF16) for _ in range(6)])
            pr = Ring([sb(ph, [128, 1024], BF16) for _ in range(8)])
            rden = sb(ph, [128, 512], F32)
            Brden = Buf()
            osb = Ring([sb(ph, [128, 512], F32) for _ in range(2)])
            rb31 = sb(ph, [128, 16], F32)
            K.dma("sp", rb31[:], rb31_d[:, :], w=[Bconst])
            sqr_ = Ring([sb(ph, [128, 512], F32) for _ in range(2)])
            rowr = Ring([sb(ph, [1, 512], F32) for _ in range(2)])
            va3 = va_d.rearrange("(kb p) c -> p kb c", p=128)
            groups = [(k, g) for k in range(4) for g in range(4)]
            gctx = {}

            def load_group(gi):
                if gi >= len(groups) or gi in gctx:
                    return
                k, g = groups[gi]
                nkb = 16 * (k + 1)
                kt, Bkt = kT.next()
                vt, Bvt = vv.next()
                qt, Bqt = qq.next()
                K.dma("sp", kt[:, 0:nkb * 128], kTa_d[g][:, 0:nkb * 128], w=[Bkt])
                K.dma("sp", vt[:, 0:nkb, :], va3[:, 0:nkb, g * 128:(g + 1) * 128], w=[Bvt])
                K.dma("sp", qt[:], qTa_d[4 * g:4 * g + 4, :, k * 512:(k + 1) * 512].rearrange("h p c -> p h c"), w=[Bqt])
                gctx[gi] = (kt, Bkt, vt, Bvt, qt, Bqt)

            items = []
            hcount = 0
            for gi, (k, g) in enumerate(groups):
                nkb = 16 * (k + 1)
                for hh in range(4):
                    for pp in range(nkb // 2):
                        items.append(dict(gi=gi, k=k, g=g, hh=hh, pp=pp, kb0=2 * pp, nkb=nkb, hpar=hcount % 2, idx=len(items)))
                    hcount += 1
            li = [0]
            mstate = {}

            def S1(it):
                gi, k, g, hh, pp, kb0, nkb = it["gi"], it["k"], it["g"], it["hh"], it["pp"], it["kb0"], it["nkb"]
                if hh == 0 and pp == 0:
                    load_group(gi)
                if hh == 0 and pp == 6:
                    load_group(gi + 1)
                kt, Bkt, vt, Bvt, qt, Bqt = gctx[gi]
                h = 4 * g + hh
                for j_ in range(it["idx"], min(it["idx"] + 5, len(items))):
                    it2 = items[j_]
                    if "mk" not in it2:
                        if it2["kb0"] % 4 == 0:
                            mkt2, Bmk2 = mk.next()
                            K.dma("sp", mkt2[:], maskT_d[it2["k"], it2["kb0"]:it2["kb0"] + 4, :, :].rearrange("a p c -> p a c"),
                                  w=[Bmk2])
                            it2["mk"] = (mkt2, Bmk2)
                        else:
                            it2["mk"] = items[j_ - 1]["mk"]
                near = kb0 >= nkb - 6
                for j_ in range(it["idx"], min(it["idx"] + 4, len(items))):
                    it2 = items[j_]
                    if it2["kb0"] >= it2["nkb"] - 6 and "bt" not in it2:
                        bt2, Bbt2 = btr.next()
                        r6 = it2["kb0"] - (it2["nkb"] - 6)
                        K.dma("sp", bt2[:], Bt_d[4 * it2["g"] + it2["hh"], r6:r6 + 2].rearrange("a p c -> p a c"), w=[Bbt2])
                        it2["bt"] = (bt2, Bbt2)
                if near:
                    bt, Bbt = it["bt"]
                zj = li[0] % 2
                li[0] += 1
                for sub in (0, 1):
                    kb = kb0 + sub
                    K.mm(ps[2 * zj + sub][:, :], kt[:, kb * 128:(kb + 1) * 128], qt[:, hh, :], True, True, [Bkt, Bqt],
                         [Bps[2 * zj + sub]])
                et, Be = er.next()
                if near:
                    tm, Btm = tmpr.next()
                    K.tt("dve", tm[:], psb[zj][:, :], bt[:].rearrange("p a b -> p (a b)"), ALU.add,
                         [Bps[2 * zj], Bps[2 * zj + 1], Bbt], [Btm])
                    K.act(et[:], tm[:], AF.Exp, [Btm], [Be])
                else:
                    K.act(et[:], psb[zj][:, :], AF.Exp, [Bps[2 * zj], Bps[2 * zj + 1], Bconst], [Be], scale=1.0,
                          bias=rb31[:, h:h + 1])
                it["e"] = (et, Be)

            def S2(it):
                et, Be = it["e"]
                mkt, Bmk = it["mk"]
                kb0 = it["kb0"]
                pt, Bp = pr.next()
                m0 = kb0 % 4
                K.tt("pool" if it["pp"] % 4 == 0 else "dve", pt[:], et[:], mkt[:, m0:m0 + 2, :].rearrange("p a b -> p (a b)"),
                     ALU.mult, [Be, Bmk], [Bp])
                it["p"] = (pt, Bp)

            def S3(it):
                gi, k, g, hh, pp, kb0, nkb = it["gi"], it["k"], it["g"], it["hh"], it["pp"], it["kb0"], it["nkb"]
                kt, Bkt, vt, Bvt, qt, Bqt = gctx[gi]
                pt, Bp = it["p"]
                psN, psD = (4, 5) if it["hpar"] == 0 else (6, 7)
                lastp = (pp == nkb // 2 - 1)
                for sub in (0, 1):
                    K.mm(ps[psN][:, :], vt[:, kb0 + sub, :], pt[:, sub * 512:(sub + 1) * 512], pp == 0 and sub == 0,
                         lastp and sub == 1, [Bvt, Bp], [Bps[psN]])
                    K.mm(ps[psD][:, :], onesb[:], pt[:, sub * 512:(sub + 1) * 512], pp == 0 and sub == 0,
                         lastp and sub == 1, [Bconst, Bp], [Bps[psD]])
                if lastp:
                    h = 4 * g + hh
                    K.op("dve", lambda e, psD=psD: e.reciprocal(out=rden[:], in_=ps[psD][:, :]), [Bps[psD]], [Brden])
                    ot, Bo = osb.next()
                    K.tt("dve", ot[:], ps[psN][:, :], rden[:], ALU.mult, [Bps[psN], Brden], [Bo])
                    K.dma("sp", oT_d[h][:, k * 512:(k + 1) * 512], ot[:], r=[Bo])
                    sq, Bsq = sqr_.next()
                    K.act(sq[:], ot[:], AF.Square, [Bo], [Bsq])
                    K.mm(ps[psD][0:1, :], onesf[:, 0:1], sq[:], True, True, [Bconst, Bsq], [Bps[psD]])
                    rw, Brw = rowr.next()
                    K.cp("act", rw[:], ps[psD][0:1, :], [Bps[psD]], [Brw])
                    K.dma("sp", ssq_d[h:h + 1, k * 512:(k + 1) * 512], rw[:], r=[Brw])

            run_pipeline([S1, S2, None, None, S3], items)
            K.barrier()

    if stop_after >= "E":
        with ExitStack() as ph:
            kT = Ring([sb(ph, [128, S], BF16) for _ in range(2)])
            vv = Ring([sb(ph, [128, 64, 128], BF16) for _ in range(2)])
            qq = Ring([sb(ph, [128, 512], BF16) for _ in range(2)])
            qnr = Ring([sb(ph, [128, 512], BF16) for _ in range(2)])
            cmr = sb(ph, [128, 4, 512], BF16)
            cmf = sb(ph, [128, 512], F32)
            tri = sb(ph, [128, 128], BF16)
            trif = sb(ph, [128, 128], F32)
            Bc2 = Buf()
            for r_ in range(4):
                K.dma("sp", cmf[:], cm_d[:, 3 - r_, :], w=[Bc2])
                K.cp("dve", cmr[:, r_, :], cmf[:], [Bc2], [Bc2])
            K.dma("sp", trif[:], tri_d[:, :], w=[Bc2])
            K.cp("dve", tri[:], trif[:], [Bc2], [Bc2])
            efr = Ring([sb(ph, [128, 1024], F32) for _ in range(2)])
            spr = Ring([sb(ph, [128, 1024], BF16) for _ in range(4)])
            sprw = Ring([sb(ph, [128, 1024], BF16) for _ in range(2)])
            ar = Ring([sb(ph, [128, 1024], BF16) for _ in range(4)])
            arw = Ring([sb(ph, [128, 1024], BF16) for _ in range(2)])
            Rr = Ring([sb(ph, [128, 512], BF16) for _ in range(5)])
            osb = Ring([sb(ph, [128, 512], F32) for _ in range(2)])
            sqr_ = Ring([sb(ph, [128, 512], F32) for _ in range(2)])
            rowr = Ring([sb(ph, [1, 512], F32) for _ in range(2)])
            vb3 = vb_d.rearrange("(kb p) c -> p kb c", p=128)
            heads = [(k, h) for k in range(4) for h in range(16)]
            hctx = {}

            def load_head(hi):
                if hi >= len(heads) or hi in hctx:
                    return
                k, h = heads[hi]
                nkb = 16 * (k + 1)
                kt, Bkt = kT.next()
                vt, Bvt = vv.next()
                qt, Bqt = qq.next()
                qn, Bqn = qnr.next()
                K.dma("sp", kt[:, 0:nkb * 128], kTb_d[h][:, 0:nkb * 128], w=[Bkt])
                K.dma("sp", vt[:, 0:nkb, :], vb3[:, 0:nkb, h * 128:(h + 1) * 128], w=[Bvt])
                K.dma("sp", qt[:], qTb_d[h][:, k * 512:(k + 1) * 512], w=[Bqt])
                K.ts("dve", qn[:], qt[:], -1.0, None, ALU.mult, None, [Bqt], [Bqn])
                hctx[hi] = (kt, Bkt, vt, Bvt, qt, Bqt, qn, Bqn)

            items = []
            for hi, (k, h) in enumerate(heads):
                nkb = 16 * (k + 1)
                npr = nkb // 2
                for p in range(npr):
                    items.append(dict(hi=hi, k=k, h=h, p=p, kb0=nkb - 1 - 2 * p, nkb=nkb, first=(p == 0), last=(p == npr - 1)))
            lti = [0]
            rstate = {}

            def S1(it):
                hi, kb0 = it["hi"], it["kb0"]
                if it["first"]:
                    load_head(hi)
                if it["p"] == 5:
                    load_head(hi + 1)
                kt, Bkt, vt, Bvt, qt, Bqt, qn, Bqn = hctx[hi]
                for sub in (0, 1):
                    kb = kb0 - sub
                    K.mm(ps[sub][:, :], kt[:, kb * 128:(kb + 1) * 128], qt[:], True, True, [Bkt, Bqt], [Bps[sub]])
                ef, Bef = efr.next()
                K.act(ef[:], psb[0][:, :], AF.Exp, [Bps[0], Bps[1]], [Bef])
                spt, Bsp = spr.next()
                if it["p"] < 2:
                    sw, Bsw = sprw.next()
                    K.act(sw[:], ef[:], AF.Ln, [Bef], [Bsw], scale=1.0, bias=1.0)
                    K.tt("pool", spt[:], sw[:], cmr[:, 2 * it["p"]:2 * it["p"] + 2, :].rearrange("p a b -> p (a b)"), ALU.mult,
                         [Bsw, Bc2], [Bsp])
                else:
                    K.act(spt[:], ef[:], AF.Ln, [Bef], [Bsp], scale=1.0, bias=1.0)
                it["sp"] = (spt, Bsp)
                it["Rprev"] = None if it["first"] else rstate["R"]
                if not it["last"]:
                    Rn, BRn = Rr.next()
                    if it["first"]:
                        K.tt("pool", Rn[:], spt[:, 0:512], spt[:, 512:1024], ALU.add, [Bsp], [BRn])
                    else:
                        Rp, BRp = rstate["R"]
                        K.tt("pool", Rn[:], Rp[:], spt[:, 0:512], ALU.add, [BRp, Bsp], [BRn])
                        K.tt("pool", Rn[:], Rn[:], spt[:, 512:1024], ALU.add, [BRn, Bsp], [BRn])
                    rstate["R"] = (Rn, BRn)

            def S2(it):
                hi, kb0 = it["hi"], it["kb0"]
                kt, Bkt, vt, Bvt, qt, Bqt, qn, Bqn = hctx[hi]
                spt, Bsp = it["sp"]
                first = it["first"]
                lj = 1 + (lti[0] % 2)
                lti[0] += 1
                it["lj"] = lj
                b0, b1 = 2 * lj, 2 * lj + 1
                K.mm(ps[b0][:, :], tri[:], spt[:, 0:512], True, False, [Bc2, Bsp], [Bps[b0]])
                if not first:
                    Rp, BRp = it["Rprev"]
                    K.mm(ps[b0][:, :], onesb[:], Rp[:], False, False, [Bconst, BRp], [Bps[b0]])
                K.mm(ps[b0][:, :], kt[:, kb0 * 128:(kb0 + 1) * 128], qn[:], False, True, [Bkt, Bqn], [Bps[b0]])
                K.mm(ps[b1][:, :], tri[:], spt[:, 512:1024], True, False, [Bc2, Bsp], [Bps[b1]])
                K.mm(ps[b1][:, :], onesb[:], spt[:, 0:512], False, False, [Bconst, Bsp], [Bps[b1]])
                if not first:
                    K.mm(ps[b1][:, :], onesb[:], Rp[:], False, False, [Bconst, BRp], [Bps[b1]])
                K.mm(ps[b1][:, :], kt[:, (kb0 - 1) * 128:kb0 * 128], qn[:], False, True, [Bkt, Bqn], [Bps[b1]])

            def S3(it):
                lj = it["lj"]
                at, Ba = ar.next()
                if it["p"] < 2:
                    aw, Baw = arw.next()
                    K.act(aw[:], psb[lj][:, :], AF.Exp, [Bps[2 * lj], Bps[2 * lj + 1]], [Baw], scale=-1.0)
                    K.tt("pool", at[:], aw[:], cmr[:, 2 * it["p"]:2 * it["p"] + 2, :].rearrange("p a b -> p (a b)"), ALU.mult,
                         [Baw, Bc2], [Ba])
                else:
                    K.act(at[:], psb[lj][:, :], AF.Exp, [Bps[2 * lj], Bps[2 * lj + 1]], [Ba], scale=-1.0)
                it["a"] = (at, Ba)

            def S4(it):
                hi, k, h, kb0 = it["hi"], it["k"], it["h"], it["kb0"]
                kt, Bkt, vt, Bvt, qt, Bqt, qn, Bqn = hctx[hi]
                at, Ba = it["a"]
                psO = 6 + (hi % 2)
                K.mm(ps[psO][:, :], vt[:, kb0, :], at[:, 0:512], it["first"], False, [Bvt, Ba], [Bps[psO]])
                K.mm(ps[psO][:, :], vt[:, kb0 - 1, :], at[:, 512:1024], False, it["last"], [Bvt, Ba], [Bps[psO]])
                if it["last"]:
                    ot, Bo = osb.next()
                    K.cp("dve", ot[:], ps[psO][:, :], [Bps[psO]], [Bo])
                    K.dma("sp", oT_d[16 + h][:, k * 512:(k + 1) * 512], ot[:], r=[Bo])
                    sq, Bsq = sqr_.next()
                    K.tt("dve", sq[:], ot[:], ot[:], ALU.mult, [Bo], [Bsq])
                    K.mm(ps[psO][0:1, :], onesf[:, 0:1], sq[:], True, True, [Bconst, Bsq], [Bps[psO]])
                    rw, Brw = rowr.next()
                    K.cp("dve", rw[:], ps[psO][0:1, :], [Bps[psO]], [Brw])
                    K.dma("sp", ssq_d[16 + h:17 + h, k * 512:(k + 1) * 512], rw[:], r=[Brw])

            run_pipeline([S1, S2, S3, S4], items)
            K.barrier()

    if stop_after >= "F":
        with ExitStack() as ph:
            onT = sb(ph, [128, 32, 1024], BF16)
            BonT = bufs(32)
            otr = Ring([sb(ph, [128, 1024], F32) for _ in range(3)])
            ssqt = sb(ph, [16, 2, 1024], F32)
            Bssq = Buf()
            rbc = [sb(ph, [128, 1024], F32) for _ in range(2)]
            Brbc = bufs(2)
            stgf = Ring([sb(ph, [128, 1024], F32) for _ in range(3)])
            pj = Proj(ph)
            W3o = w3(w_out)
            for g in range(2):
                cols = slice(g * 1024, (g + 1) * 1024)
                for ab in (0, 1):
                    K.dma("sp", ssqt[0:16, ab, :], ssq_d[16 * ab:16 * ab + 16, cols], w=[Bssq])
                for ab in (0, 1):
                    for h in (0, 1):
                        pb = 2 * ab + h
                        K.mm(ps[pb][:, :], onesf[0:16, :], ssqt[0:16, ab, h * 512:(h + 1) * 512], True, True,
                             [Bconst, Bssq], [Bps[pb]])
                for ab in (0, 1):
                    for h in (0, 1):
                        pb = 2 * ab + h
                        K.act(rbc[ab][:, h * 512:(h + 1) * 512], ps[pb][:, :], AF.Sqrt, [Bps[pb]], [Brbc[ab]],
                              scale=1.0 / 2048.0, bias=EPS)
                    K.op("dve", lambda e, ab=ab: e.reciprocal(out=rbc[ab][:], in_=rbc[ab][:]), [Brbc[ab]], [Brbc[ab]])
                for j in range(32):
                    ot, Bo = otr.next()
                    K.dma("sp", ot[:], oT_d[j][:, cols], w=[Bo])
                    K.stt("dve", onT[:, j, :], ot[:], prmT[:, 4, j:j + 1], rbc[j // 16][:], ALU.mult,
                          ALU.mult, [Bo, Bconst, Brbc[j // 16]], [BonT[j]])

                def epi_mix(n_off, g=g):
                    def epi(pss, Bpss, idx):
                        s_, Bs = stgf.next()
                        n = idx
                        for h in (0, 1):
                            if (idx + h) % 2 == 0:
                                K.act(s_[:, h * 512:(h + 1) * 512], pss[h][:, :], AF.Identity, [Bpss[h], Bmod], [Bs],
                                      scale=modT[:, 64 + n:65 + n])
                            else:
                                K.ts("dve", s_[:, h * 512:(h + 1) * 512], pss[h][:, :], modT[:, 64 + n:65 + n], None,
                                     ALU.mult, None, [Bpss[h], Bmod], [Bs])
                        K.dma("sp", mixT_d[n][:, g * 1024:(g + 1) * 1024], s_[:], r=[Bs])
                        return None
                    return epi

                tiles = [(n * 128, 128, epi_mix(n)) for n in range(32)]
                pj.run(W3o, tiles, onT, lambda kc, h: [BonT[kc]], (0, 1), npairs=4)
            K.barrier()

    def res_ln_pass(ph, final):
        npart = 4 if final else 1
        part = [Ring([sb(ph, [128, 16, 128], F32) for _ in range(2 if final else 4)]) for _ in range(npart)]
        xr = Ring([sb(ph, [128, D], F32) for _ in range(2)])
        hpre = Ring([sb(ph, [128, D], F32) for _ in range(2)])
        gbc = sb(ph, [128, D], F32)
        bbc = sb(ph, [128, D], F32)
        Bgb = Buf()
        stats = sb(ph, [128, 48], F32)
        st = sb(ph, [128, 8], F32)
        Bst = Buf()
        stats2 = sb(ph, [128, 48], F32)
        st2 = sb(ph, [128, 8], F32)
        Bst2 = Buf()
        if not final:
            g0 = sb(ph, [128, D], F32)
            b0 = sb(ph, [128, D], F32)
            K.dma("sp", g0[:], bc_d[0], w=[Bgb])
            K.dma("sp", b0[:], bc_d[1], w=[Bgb])
            K.dma("sp", gbc[:], bc_d[2], w=[Bgb])
            K.dma("sp", bbc[:], bc_d[3], w=[Bgb])
            xhb = Ring([sb(ph, [128, D], BF16) for _ in range(2)])
            u2s = Ring([sb(ph, [128, 32, 128], BF16) for _ in range(2)])
        else:
            K.dma("sp", gbc[:], bc_d[4], w=[Bgb])
            K.dma("sp", bbc[:], bc_d[5], w=[Bgb])
        state = {}

        def stage_A(tb):
            k, r_ = tb // 4, tb % 4
            tcols = slice(tb * 128, (tb + 1) * 128)
            halves = []
            for hf in range(2):
                pts = []
                for q in range(npart):
                    pt, Bpt = part[q].next()
                    src = (yT_d[q] if final else mixT_d)[hf * 16:(hf + 1) * 16, :, tcols].rearrange("n p c -> p n c")
                    K.dma("sp", pt[:], src, w=[Bpt])
                    pts.append((pt, Bpt))
                halves.append(pts)
                if hf == 0:
                    xt, Bx = xr.next()
                    if final:
                        K.dma("sp", xt[:], h1_d[tb * 128:(tb + 1) * 128, :], w=[Bx])
                    else:
                        r0 = (4 * k + 3) * 512 + r_ * 128
                        K.dma("sp", xt[:], xpad[r0:r0 + 128, :], w=[Bx])
            if not final:
                ln_stats(xt, Bx, stats, st, Bst)
                K.act(xt[:], xt[:], AF.Identity, [Bx, Bst], [Bx], scale=st[:, 4:5], bias=st[:, 5:6])
                K.tt("dve", xt[:], xt[:], g0[:], ALU.mult, [Bx, Bgb], [Bx])
                K.tt("pool", xt[:], xt[:], b0[:], ALU.add, [Bx, Bgb], [Bx])
            state[tb] = (halves, xt, Bx)

        def stage_B(tb):
            tcols = slice(tb * 128, (tb + 1) * 128)
            halves, resid, Bres = state.pop(tb)
            hp, Bhp = hpre.next()
            for n4 in range(8):
                pb = n4
                pts = halves[n4 // 4]
                for i in range(4):
                    n = (n4 % 4) * 4 + i
                    for q in range(npart):
                        K.mm(ps[pb][:, i * 128:(i + 1) * 128], pts[q][0][:, n, :], identf[:], q == 0, q == npart - 1,
                             [pts[q][1], Bconst], [Bps[pb]])
                K.stt("dve", hp[:, n4 * 512:(n4 + 1) * 512], resid[:, n4 * 512:(n4 + 1) * 512], ALPHA, ps[pb][:, :],
                      ALU.mult, ALU.add, [Bres, Bps[pb]], [Bhp])
            ln_stats(hp, Bhp, stats2, st2, Bst2)
            ot, Bo = hp, Bhp
            if not final:
                xb, Bxb = xhb.next()
                K.act(xb[:], hp[:], AF.Identity, [Bhp, Bst2], [Bxb], scale=st2[:, 4:5], bias=st2[:, 5:6])
            K.act(ot[:], hp[:], AF.Identity, [Bhp, Bst2], [Bo], scale=st2[:, 4:5], bias=st2[:, 5:6])
            K.tt("dve", ot[:], ot[:], gbc[:], ALU.mult, [Bo, Bgb], [Bo])
            K.tt("pool", ot[:], ot[:], bbc[:], ALU.add, [Bo, Bgb], [Bo])
            if final:
                K.dma("sp", out_d[tb * 128:(tb + 1) * 128, :], ot[:], r=[Bo])
            else:
                K.dma("sp", h1_d[tb * 128:(tb + 1) * 128, :], ot[:], r=[Bo])
                u2, Bu2 = u2s.next()
                to_featmajor(xb, Bxb, AB[:, 2, :], AB[:, 3, :], lambda kc, u2=u2: u2[:, kc, :], lambda kc, Bu2=Bu2: Bu2,
                             [0, 1, 2, 3])
                K.dma("sp", u2T_d[:, :, tcols], u2[:], r=[Bu2])

        if final:
            for tb in range(16):
                stage_A(tb)
                stage_B(tb)
        else:
            run_pipeline([stage_A, stage_B], list(range(16)))

    if stop_after >= "F":
        with ExitStack() as ph:
            res_ln_pass(ph, False)
            K.barrier()

    if stop_after >= "G":
        with ExitStack() as ph:
            u2T = sb(ph, [128, 32, 1024], BF16)
            Bu2T = Buf()
            hT = sb(ph, [128, 32, 1024], BF16)
            BhT = bufs(32)
            rl = Ring([sb(ph, [128, 512], F32) for _ in range(4)])
            stgf = Ring([sb(ph, [128, 1024], F32) for _ in range(2)])
            pj = Proj(ph)
            for g in range(2):
                K.dma("sp", u2T[:], u2T_d[:, :, g * 1024:(g + 1) * 1024], w=[Bu2T])
                for q in range(4):
                    def epi_up(pss, Bpss, idx):
                        for h in (0, 1):
                            rt, Br = rl.next()
                            K.act(rt[:], pss[h][:, :], AF.Relu, [Bpss[h]], [Br])
                            K.tt("pool" if h == 0 else "dve", hT[:, idx, h * 512:(h + 1) * 512], rt[:], rt[:], ALU.mult, [Br],
                                 [BhT[idx]])
                        return None

                    def epi_dn(q=q, g=g):
                        def epi(pss, Bpss, idx):
                            s_, Bs = stgf.next()
                            n = idx
                            for h in (0, 1):
                                if (idx + h) % 2 == 0:
                                    K.act(s_[:, h * 512:(h + 1) * 512], pss[h][:, :], AF.Identity, [Bpss[h], Bmod], [Bs],
                                          scale=modT[:, 160 + n:161 + n])
                                else:
                                    K.ts("dve", s_[:, h * 512:(h + 1) * 512], pss[h][:, :], modT[:, 160 + n:161 + n], None,
                                         ALU.mult, None, [Bpss[h], Bmod], [Bs])
                            K.dma("sp", yT_d[q, n][:, g * 1024:(g + 1) * 1024], s_[:], r=[Bs])
                            return None
                        return epi

                    W3u = w3(w_up)
                    tiles = [(q * 4096 + n * 128, 128, epi_up) for n in range(32)]
                    pj.run(W3u, tiles, u2T, lambda kc, h: [Bu2T], (0, 1), npairs=4)
                    W3d = w3(w_down, q * 4096)
                    e_dn = epi_dn()
                    tiles = [(n * 128, 128, e_dn) for n in range(32)]
                    pj.run(W3d, tiles, hT, lambda kc, h: [BhT[kc]], (0, 1), npairs=4)
            K.barrier()
        with ExitStack() as ph:
            res_ln_pass(ph, True)
            K.barrier()

    K.barrier()
    K.finish()
    G.close()
    return nc, K


def _t5_bucket(n):
    n = np.maximum(n, 0)
    nf = np.maximum(n, 1).astype(np.float32)
    large = 16 + (np.log(nf / np.float32(16)) / np.float32(math.log(128 / 16)) * np.float32(16)).astype(np.int32)
    large = np.minimum(large, 31)
    return np.where(n < 16, n, large)


def _consts():
    identf = np.eye(128, dtype=np.float32)
    j = np.arange(128)[:, None]
    s = np.arange(128)[None, :]
    tri = (j >= s).astype(np.float32)
    t = np.arange(128)[:, None]
    sl = np.arange(512)[None, :]
    cneg = np.stack([np.where(sl > r * 128 + t, -BIG, 0.0) for r in range(4)]).astype(np.float32)
    sk = np.arange(128)[:, None]
    tq = np.arange(512)[None, :]
    cm = np.stack([(r * 128 + sk < tq) for r in range(4)], axis=1).astype(np.float32)
    return identf, tri, cneg, np.ascontiguousarray(cm)


def _bias_tiles(rel_bias):
    sk = np.arange(128)[:, None]
    tq = np.arange(512)[None, :]
    out = np.empty((16, 6, 128, 512), np.float32)
    for r6 in range(6):
        r = r6 - 1
        dist = (1 - r) * 128 + tq - sk
        bk = _t5_bucket(dist.astype(np.int32))
        out[:, r6] = np.transpose(rel_bias[bk], (2, 0, 1))
    return out


def make_in_maps(x, c, in_ln_g, in_ln_b, rel_bias, w_ada, b_ada, w_in, idx_kn_g, idx_kn_b, gn_sparse_g, gn_sb_g,
                 w_out, ln1_g, ln1_b, w_up, w_down, ln2_g, ln2_b):
    f = lambda a: np.ascontiguousarray(np.asarray(a, dtype=np.float32))
    x = f(x)
    identf, tri, cneg, cm = _consts()
    T32 = lambda v: np.ascontiguousarray(f(v).reshape(-1, 128).T)
    prmT = np.ascontiguousarray(np.stack([T32(in_ln_g), T32(in_ln_b), T32(ln1_g[0]), T32(ln1_b[0]),
                                          T32(np.concatenate([f(gn_sparse_g[0]), f(gn_sb_g[0])]))], axis=1))
    bc = np.ascontiguousarray(np.stack([np.broadcast_to(f(v).reshape(1, D), (128, D)) for v in
                                        (in_ln_g, in_ln_b, ln1_g[0], ln1_b[0], ln2_g[0], ln2_b[0])]))
    ikgb = np.ascontiguousarray(np.concatenate([np.broadcast_to(f(idx_kn_g[0]).reshape(1, 64), (128, 64)),
                                                np.broadcast_to(f(idx_kn_b[0]).reshape(1, 64), (128, 64))], axis=1))
    rel_bias = f(rel_bias)
    rb31 = np.ascontiguousarray(np.broadcast_to(rel_bias[31].reshape(1, 16), (128, 16)))
    Bt = _bias_tiles(rel_bias)
    shared = dict(badaT=T32(b_ada[0]), prmT=prmT, bc=bc, ikgb=ikgb, rb31=rb31, Bt=Bt, identf=identf, tri=tri,
                  cneg=cneg, cm=cm, w_ada=f(w_ada[0]), w_in=f(w_in[0]), w_out=f(w_out[0]), w_up=f(w_up[0]),
                  w_down=f(w_down[0]))
    maps = []
    for core in range(8):
        b, j = core // 4, core % 4
        xp = np.zeros((S, D), np.float32)
        kval = np.zeros((S,), np.float32)
        for p in range(16):
            cch = p - 3 + j
            if cch >= 0:
                xp[p * 512:(p + 1) * 512] = x[b, cch * 512:(cch + 1) * 512]
                kval[p * 512:(p + 1) * 512] = 1.0
        m = dict(shared)
        m.update(xpad=xp, kvalT=np.ascontiguousarray(kval.reshape(64, 128).T), kvrow=kval.reshape(1, S).copy(),
                 cT=T32(f(c)[b]))
        maps.append(m)
    return maps


_CACHE = {}


def kernel(**inputs):
    maps = make_in_maps(**inputs)
    if "nc" not in _CACHE:
        _CACHE["nc"] = build()[0]
    res = run_bass_kernel_spmd(_CACHE["nc"], maps, core_ids=list(range(8)))
    out = np.empty((2, S, D), np.float32)
    for core in range(8):
        b, j = core // 4, core % 4
        o = res.results[core]["out"]
        for k in range(4):
            cch = 4 * k + j
            out[b, cch * 512:(cch + 1) * 512] = o[k * 512:(k + 1) * 512]
    return out
```

```python
from contextlib import ExitStack
import math
import numpy as np
import concourse.bass as bass
import concourse.mybir as mybir
from concourse.bass_utils import run_bass_kernel_spmd

F32 = mybir.dt.float32
BF16 = mybir.dt.bfloat16
ALU = mybir.AluOpType
AF = mybir.ActivationFunctionType

D = 4096
S = 8192
DFF = 16384
IN_COLS = 11360
EPS = 1e-5
ALPHA = 2.0 ** 0.25
BIG = 30000.0
NBIS = 24
C_AQ, C_AK, C_AV, C_BQ, C_BK, C_BV, C_IQ, C_IK = 0, 2048, 2560, 3072, 5120, 7168, 9216, 11264

ENGS = ("pe", "act", "dve", "pool", "sp")
SEM_EPOCH = 30000
NDMA = 8


class Buf:
    __slots__ = ("w", "r")

    def __init__(self):
        self.w = {}
        self.r = {}


def bufs(n):
    return [Buf() for _ in range(n)]


class KB:
    def __init__(self, nc):
        self.nc = nc
        self.es = ExitStack()
        self.sems = []
        self.prog = {e: [] for e in ENGS}
        self.cur = {}
        self.cnt = {}
        self.waited = {e: {} for e in ENGS}
        for e in ENGS:
            self.cur[e] = self._newsem()
            self.cnt[e] = 0
        self.dpool = {q: [self._newsem() for _ in range(NDMA)] for q in ("sp", "pool")}
        self.dval = {q: [0] * NDMA for q in ("sp", "pool")}
        self.dnext = {q: 0 for q in ("sp", "pool")}
        self.ninst = 0

    def _newsem(self):
        s = self.es.enter_context(self.nc.semaphore())
        self.sems.append(s)
        return len(self.sems) - 1

    def _wait(self, eng, needs):
        wd = self.waited[eng]
        for s, v in needs.items():
            if wd.get(s, 0) >= v:
                continue
            wd[s] = v
            sem = self.sems[s]
            self.prog[eng].append(lambda e, sem=sem, v=v: e.wait_ge(sem, v))

    def _needs(self, eng, r, w):
        needs = {}
        own = self.cur[eng]
        for b in r:
            for s, v in b.w.items():
                if needs.get(s, 0) < v:
                    needs[s] = v
        for b in w:
            for s, v in b.w.items():
                if s != own and needs.get(s, 0) < v:
                    needs[s] = v
            for s, v in b.r.items():
                if s != own and needs.get(s, 0) < v:
                    needs[s] = v
        return needs

    def _commit(self, s, v, r, w):
        for b in w:
            b.w = {s: v}
            b.r = {}
        for b in r:
            if b.r.get(s, 0) < v:
                b.r[s] = v

    def op(self, eng, fn, r=(), w=()):
        if self.cnt[eng] >= SEM_EPOCH:
            self.cur[eng] = self._newsem()
            self.cnt[eng] = 0
        self._wait(eng, self._needs(eng, r, w))
        s = self.cur[eng]
        self.cnt[eng] += 1
        v = self.cnt[eng]
        sem = self.sems[s]
        self.prog[eng].append(lambda e, fn=fn, sem=sem: fn(e).then_inc(sem, 1))
        self._commit(s, v, r, w)
        self.ninst += 1

    def dma(self, q, out, in_, r=(), w=()):
        i = self.dnext[q]
        self.dnext[q] = (i + 1) % NDMA
        s = self.dpool[q][i]
        needs = self._needs(q, r, w)
        if self.dval[q][i] > 0:
            needs[s] = max(needs.get(s, 0), self.dval[q][i])
        self._wait(q, needs)
        self.dval[q][i] += 16
        v = self.dval[q][i]
        sem = self.sems[s]
        self.prog[q].append(lambda e, out=out, in_=in_, sem=sem: e.dma_start(out=out, in_=in_).then_inc(sem, 16))
        self._commit(s, v, r, w)
        self.ninst += 1

    def barrier(self):
        latest = {}
        for e in ENGS:
            if self.cnt[e] > 0:
                latest[self.cur[e]] = self.cnt[e]
        for q in self.dpool:
            for i, s in enumerate(self.dpool[q]):
                if self.dval[q][i] > 0:
                    latest[s] = self.dval[q][i]
        for e in ENGS:
            self._wait(e, dict(latest))

    def finish(self):
        prog = self.prog
        with self.nc.Block() as block:
            @block.sync
            def _(e):
                for f in prog["sp"]:
                    f(e)

            @block.tensor
            def _(e):
                for f in prog["pe"]:
                    f(e)

            @block.scalar
            def _(e):
                for f in prog["act"]:
                    f(e)

            @block.vector
            def _(e):
                for f in prog["dve"]:
                    f(e)

            @block.gpsimd
            def _(e):
                for f in prog["pool"]:
                    f(e)
        self.es.close()

    def mm(self, out, lhsT, rhs, start, stop, r, w):
        self.op("pe", lambda e: e.matmul(out, lhsT=lhsT, rhs=rhs, start=start, stop=stop), r, w)

    def act(self, out, in_, func, r, w, scale=1.0, bias=0.0):
        self.op("act", lambda e: e.activation(out=out, in_=in_, func=func, bias=bias, scale=scale), r, w)

    def ts(self, eng, out, in0, s1, s2, op0, op1, r, w, accum=None):
        if op1 is None:
            self.op(eng, lambda e: e.tensor_scalar(out=out, in0=in0, scalar1=s1, scalar2=None, op0=op0), r, w)
        elif accum is None:
            self.op(eng, lambda e: e.tensor_scalar(out=out, in0=in0, scalar1=s1, scalar2=s2, op0=op0, op1=op1), r, w)
        else:
            self.op(eng, lambda e: e.tensor_scalar(out=out, in0=in0, scalar1=s1, scalar2=s2, op0=op0, op1=op1,
                                                   accum_out=accum), r, w)

    def tt(self, eng, out, in0, in1, op, r, w):
        self.op(eng, lambda e: e.tensor_tensor(out=out, in0=in0, in1=in1, op=op), r, w)

    def stt(self, eng, out, in0, scalar, in1, op0, op1, r, w):
        self.op(eng, lambda e: e.scalar_tensor_tensor(out=out, in0=in0, scalar=scalar, in1=in1, op0=op0, op1=op1), r, w)

    def cp(self, eng, out, in_, r, w):
        if eng == "act":
            self.op("act", lambda e: e.activation(out=out, in_=in_, func=AF.Copy), r, w)
        else:
            self.op(eng, lambda e: e.tensor_copy(out=out, in_=in_), r, w)


class Ring:
    def __init__(self, tiles):
        self.t = tiles
        self.b = bufs(len(tiles))
        self.i = 0

    def next(self):
        i = self.i
        self.i = (i + 1) % len(self.t)
        return self.t[i], self.b[i]


def build(stop_after="G", debug=False):
    nc = bass.Bass("TRN2", target_bir_lowering=False)
    K = KB(nc)
    names = [0]

    def din(name, shape, dt=F32):
        return nc.dram_tensor(name, list(shape), dt, kind="ExternalInput").ap()

    def dscr(name, shape, dt):
        kind = "ExternalOutput" if (debug and name in debug) else "Internal"
        return nc.dram_tensor(name, list(shape), dt, kind=kind).ap()

    def sb(stack, shape, dt):
        names[0] += 1
        return stack.enter_context(nc.sbuf_tensor(f"sb{names[0]}", list(shape), dt))

    xpad = din("xpad", [S, D])
    kvalT_d = din("kvalT", [128, 64])
    kvrow_d = din("kvrow", [1, S])
    cT_d = din("cT", [128, 32])
    badaT_d = din("badaT", [128, 192])
    prmT_d = din("prmT", [128, 5, 32])
    bc_d = din("bc", [6, 128, D])
    ikgb_d = din("ikgb", [128, 128])
    rb31_d = din("rb31", [128, 16])
    Bt_d = din("Bt", [16, 6, 128, 512])
    identf_d = din("identf", [128, 128])
    tri_d = din("tri", [128, 128])
    cneg_d = din("cneg", [4, 128, 512])
    cm_d = din("cm", [128, 4, 512])
    w_ada = din("w_ada", [D, 6 * D])
    w_in = din("w_in", [D, IN_COLS])
    w_out = din("w_out", [D, D])
    w_up = din("w_up", [D, DFF])
    w_down = din("w_down", [DFF, D])
    out_d = nc.dram_tensor("out", [2048, D], F32, kind="ExternalOutput").ap()

    kTb_d = dscr("kTb", [16, 128, S], BF16)
    kTa_d = dscr("kTa", [4, 128, S], BF16)
    vb_d = dscr("vb", [S, 2048], BF16)
    va_d = dscr("va", [S, 512], BF16)
    ikT_d = dscr("ikT", [64, S], BF16)
    iw_d = dscr("iw", [S, 32], F32)
    qTa_d = dscr("qTa", [16, 128, 2048], BF16)
    qTb_d = dscr("qTb", [16, 128, 2048], BF16)
    iqT_d = dscr("iqT", [16, 128, 2048], BF16)
    maskT_d = dscr("maskT", [4, 64, 128, 512], BF16)
    oT_d = dscr("oT", [32, 128, 2048], F32)
    ssq_d = dscr("ssq", [32, 2048], F32)
    mixT_d = dscr("mixT", [16, 128, 32, 128], F32)
    h1_d = dscr("h1", [2048, D], F32)
    u2T_d = dscr("u2T", [128, 32, 2048], BF16)
    yT_d = dscr("yT", [4, 16, 128, 32, 128], F32)
    modT_dbg = dscr("modT", [128, 192], F32) if (debug and "modT" in debug) else None

    G = ExitStack()
    psb = [G.enter_context(nc.psum_tensor(f"psb{i}", [128, 1024], F32)) for i in range(4)]
    ps = []
    for j_ in range(4):
        ps += [psb[j_][:, 0:512], psb[j_][:, 512:1024]]
    Bps = bufs(8)

    identf = sb(G, [128, 128], F32)
    identb = sb(G, [128, 128], BF16)
    onesb = sb(G, [128, 128], BF16)
    onesf = sb(G, [128, 128], F32)
    modT = sb(G, [128, 192], F32)
    prmT = sb(G, [128, 5, 32], F32)
    AB = sb(G, [128, 4, 32], F32)
    kvalT = sb(G, [128, 64], F32)
    Bconst = Buf()
    Bmod = Buf()

    K.dma("sp", identf[:], identf_d[:, :], w=[Bconst])
    K.dma("sp", prmT[:], prmT_d[:, :, :], w=[Bconst])
    K.dma("sp", kvalT[:], kvalT_d[:, :], w=[Bconst])
    K.cp("dve", identb[:], identf[:], [Bconst], [Bconst])
    K.op("dve", lambda e: e.memset(onesb[:], 1.0), w=[Bconst])
    K.op("dve", lambda e: e.memset(onesf[:], 1.0), w=[Bconst])

    with ExitStack() as ph:
        cT = sb(ph, [128, 32], F32)
        cs = sb(ph, [128, 32], F32)
        badaT = sb(ph, [128, 192], F32)
        row = [sb(ph, [1, 2048], F32) for _ in range(2)]
        Brow = bufs(2)
        wa = Ring([sb(ph, [128, 2048], F32) for _ in range(4)])
        Bc = Buf()
        K.dma("sp", cT[:], cT_d[:, :], w=[Bc])
        K.dma("sp", badaT[:], badaT_d[:, :], w=[Bc])
        K.act(cs[:], cT[:], AF.Silu, [Bc], [Bc])
        psM, BpsM = ps[4], Bps[4]
        for grp in range(12):
            for kc in range(32):
                wt, Bw = wa.next()
                K.dma("sp", wt[:], w_ada[kc * 128:(kc + 1) * 128, grp * 2048:(grp + 1) * 2048], w=[Bw])
                for n in range(4):
                    K.mm(ps[n][0:1, :], cs[:, kc:kc + 1], wt[:, n * 512:(n + 1) * 512], kc == 0, kc == 31,
                         [Bc, Bw], [Bps[n]])
            rw, Br = row[grp % 2], Brow[grp % 2]
            for n in range(4):
                K.cp("act" if n % 2 == 0 else "dve", rw[0:1, n * 512:(n + 1) * 512], ps[n][0:1, :], [Bps[n]], [Br])
            for jj in range(16):
                col = grp * 16 + jj
                K.mm(psM[:, col:col + 1], rw[0:1, jj * 128:(jj + 1) * 128], onesf[0:1, 0:1], True, True,
                     [Br, Bconst], [BpsM])
        K.tt("dve", modT[:], psM[:, 0:192], badaT[:], ALU.add, [BpsM, Bc], [Bmod])
        K.stt("dve", AB[:, 0, :], modT[:, 32:64], 1.0, prmT[:, 0, :], ALU.add, ALU.mult, [Bmod, Bconst], [Bmod])
        K.stt("dve", AB[:, 1, :], modT[:, 32:64], 1.0, prmT[:, 1, :], ALU.add, ALU.mult, [Bmod, Bconst], [Bmod])
        K.tt("dve", AB[:, 1, :], AB[:, 1, :], modT[:, 0:32], ALU.add, [Bmod], [Bmod])
        K.stt("dve", AB[:, 2, :], modT[:, 128:160], 1.0, prmT[:, 2, :], ALU.add, ALU.mult, [Bmod, Bconst], [Bmod])
        K.stt("dve", AB[:, 3, :], modT[:, 128:160], 1.0, prmT[:, 3, :], ALU.add, ALU.mult, [Bmod, Bconst], [Bmod])
        K.tt("dve", AB[:, 3, :], AB[:, 3, :], modT[:, 96:128], ALU.add, [Bmod], [Bmod])
        if modT_dbg is not None:
            K.dma("sp", modT_dbg[:, :], modT[:], r=[Bmod])
        K.barrier()

    def ln_stats(xt, Bx, stats, st, Bst):
        for i in range(8):
            K.op("dve", lambda e, i=i: e.bn_stats(out=stats[:, i * 6:(i + 1) * 6], in_=xt[:, i * 512:(i + 1) * 512]),
                 [Bx], [Bst])
        K.op("dve", lambda e: e.bn_aggr(out=st[:, 0:2], in_=stats[:, 0:48]), [Bst], [Bst])
        K.act(st[:, 2:3], st[:, 1:2], AF.Sqrt, [Bst], [Bst], scale=1.0, bias=EPS)
        K.op("dve", lambda e: e.reciprocal(out=st[:, 4:5], in_=st[:, 2:3]), [Bst], [Bst])
        K.stt("dve", st[:, 5:6], st[:, 0:1], -1.0, st[:, 4:5], ALU.mult, ALU.mult, [Bst], [Bst])

    def to_featmajor(xh, Bxh, Acol, Bcol, dst_fn, Bdst_fn, psbanks):
        for k4 in range(8):
            pb = psbanks[k4 % len(psbanks)]
            for i in range(4):
                kc = k4 * 4 + i
                K.mm(ps[pb][:, i * 128:(i + 1) * 128], xh[:, kc * 128:(kc + 1) * 128], identb[:], True, True,
                     [Bxh, Bconst], [Bps[pb]])
            for i in range(4):
                kc = k4 * 4 + i
                if k4 % 2 == 0:
                    K.act(dst_fn(kc), ps[pb][:, i * 128:(i + 1) * 128], AF.Identity, [Bps[pb], Bmod], [Bdst_fn(kc)],
                          scale=Acol[:, kc:kc + 1], bias=Bcol[:, kc:kc + 1])
                else:
                    K.ts("dve", dst_fn(kc), ps[pb][:, i * 128:(i + 1) * 128], Acol[:, kc:kc + 1], Bcol[:, kc:kc + 1],
                         ALU.mult, ALU.add, [Bps[pb], Bmod], [Bdst_fn(kc)])

    class Proj:
        def __init__(self, stack):
            self.wt = [sb(stack, [128, 32, 128], BF16) for _ in range(3)]
            self.Bwt = [bufs(4) for _ in range(3)]
            self.pair = 0
            self.pending = None

        def run(self, W3, tiles, mov, movbufs, halves, npairs=3, hooks=None):
            n = len(tiles)

            def load(i):
                c0, width, _ = tiles[i]
                sl = i % 3
                for q in range(4):
                    K.dma("pool", self.wt[sl][:, 8 * q:8 * q + 8, 0:width], W3[:, 8 * q:8 * q + 8, c0:c0 + width],
                          w=[self.Bwt[sl][q]])

            load(0)
            if n > 1:
                load(1)
            for i in range(n):
                c0, width, epi = tiles[i]
                sl = i % 3
                if i + 2 < n:
                    load(i + 2)
                p = self.pair
                self.pair = (p + 1) % npairs
                pss = {h: ps[2 * p + hi] for hi, h in enumerate(halves)}
                Bpss = {h: Bps[2 * p + hi] for hi, h in enumerate(halves)}
                for kc in range(32):
                    for h in halves:
                        K.mm(pss[h][0:width, :], self.wt[sl][:, kc, 0:width], mov[:, kc, h * 512:(h + 1) * 512],
                             kc == 0, kc == 31, [self.Bwt[sl][kc // 8]] + movbufs(kc, h), [Bpss[h]])
                if hooks and i in hooks:
                    hooks[i]()
                if self.pending is not None:
                    self.pending()
                    self.pending = None
                self.pending = epi(pss, Bpss, i)
            if self.pending is not None:
                self.pending()
                self.pending = None

    def w3(w, r0=0):
        return w[r0:r0 + D, :].rearrange("(kc p) c -> p kc c", p=128)

    if stop_after >= "B":
        with ExitStack() as ph:
            uTs = [sb(ph, [128, 32, 1024], BF16) for _ in range(2)]
            BuTs = [[bufs(8) for _ in range(8)] for _ in range(2)]
            xr = Ring([sb(ph, [128, D], F32) for _ in range(1)])
            xhr = Ring([sb(ph, [128, D], BF16) for _ in range(1)])
            stats = sb(ph, [128, 48], F32)
            st = sb(ph, [128, 8], F32)
            Bst = Buf()
            stg = Ring([sb(ph, [128, 1024], BF16) for _ in range(3)])
            vtok = Ring([sb(ph, [128, 8, 128], BF16) for _ in range(2)])
            ikst = sb(ph, [128, 1024], F32)
            Bikst = Buf()
            ikt = sb(ph, [128, 96], F32)
            ikn = sb(ph, [128, 64], F32)
            iknb = sb(ph, [128, 64], BF16)
            ikstats = sb(ph, [128, 6], F32)
            ist = sb(ph, [128, 8], F32)
            Bik = Buf()
            iwt = sb(ph, [128, 8, 32], F32)
            Biw = Buf()
            ikTs = sb(ph, [64, 1024], BF16)
            BikTs = Buf()
            ikgb = sb(ph, [128, 128], F32)
            K.dma("sp", ikgb[:], ikgb_d[:, :], w=[Bconst])
            pj = Proj(ph)
            W3in = w3(w_in)
            xrot = [6, 7]
            xri = [0]

            def nextx():
                b = xrot[xri[0] % 2]
                xri[0] += 1
                return b

            b1state = {}

            def emit_B1a(g, tb):
                xt, Bx = xr.next()
                xh, Bxh = xhr.next()
                r0 = g * 1024 + tb * 128
                K.dma("sp", xt[:], xpad[r0:r0 + 128, :], w=[Bx])
                ln_stats(xt, Bx, stats, st, Bst)
                K.act(xh[:], xt[:], AF.Identity, [Bx, Bst], [Bxh], scale=st[:, 4:5], bias=st[:, 5:6])
                b1state[(g, tb)] = (xh, Bxh)

            def emit_B1b(g, tb):
                uT, BuT = uTs[g % 2], BuTs[g % 2]
                xh, Bxh = b1state.pop((g, tb))
                to_featmajor(xh, Bxh, AB[:, 0, :], AB[:, 1, :],
                             lambda kc, tb=tb: uT[:, kc, tb * 128:(tb + 1) * 128],
                             lambda kc, tb=tb: BuT[tb][kc // 4], [6, 7])

            def emit_B1(g):
                for tb in range(8):
                    emit_B1a(g, tb)
                    emit_B1b(g, tb)

            emit_B1(0)
            for g in range(8):
                uT, BuT = uTs[g % 2], BuTs[g % 2]

                def movbufs(kc, h, BuT=BuT):
                    return [BuT[tb][kc // 4] for tb in range(h * 4, h * 4 + 4)]

                def epi_kT(dst, scale, halves, dcol):
                    def epi(pss, Bpss, idx):
                        s_, Bs = stg.next()
                        for hi, h in enumerate(halves):
                            if (idx + hi) % 2 == 0:
                                K.act(s_[:, hi * 512:(hi + 1) * 512], pss[h][:, :], AF.Copy, [Bpss[h]], [Bs], scale=scale)
                            else:
                                K.ts("dve", s_[:, hi * 512:(hi + 1) * 512], pss[h][:, :], scale, None, ALU.mult, None,
                                     [Bpss[h]], [Bs])
                        n = 512 * len(halves)
                        K.dma("sp", dst[:, dcol:dcol + n], s_[:, 0:n], r=[Bs])
                        return None
                    return epi

                def epi_v(dst3, ccol, g=g):
                    def epi(pss, Bpss, idx):
                        s_, Bs = stg.next()
                        for h in (0, 1):
                            if (idx + h) % 2 == 0:
                                K.act(s_[:, h * 512:(h + 1) * 512], pss[h][:, :], AF.Copy, [Bpss[h]], [Bs])
                            else:
                                K.cp("dve", s_[:, h * 512:(h + 1) * 512], pss[h][:, :], [Bpss[h]], [Bs])

                        def deferred():
                            vt, Bv = vtok.next()
                            for hb in range(2):
                                pb = nextx()
                                for i in range(4):
                                    tb = hb * 4 + i
                                    K.mm(ps[pb][:, i * 128:(i + 1) * 128], s_[:, tb * 128:(tb + 1) * 128], identb[:],
                                         True, True, [Bs, Bconst], [Bps[pb]])
                                for i in range(4):
                                    tb = hb * 4 + i
                                    blk = g * 8 + tb
                                    K.ts("dve", vt[:, tb, :], ps[pb][:, i * 128:(i + 1) * 128], kvalT[:, blk:blk + 1], None,
                                         ALU.mult, None, [Bps[pb], Bconst], [Bv])
                            K.dma("sp", dst3[:, g * 8:(g + 1) * 8, ccol:ccol + 128], vt[:], r=[Bv])
                        return deferred
                    return epi

                def epi_ik(pss, Bpss, idx, g=g):
                    for h in (0, 1):
                        K.cp("act", ikst[0:96, h * 512:(h + 1) * 512], pss[h][0:96, :], [Bpss[h]], [Bikst])

                    def deferred():
                        for tb in range(8):
                            pb = nextx()
                            K.mm(ps[pb][:, 0:96], ikst[0:96, tb * 128:(tb + 1) * 128], identf[0:96, 0:96], True, True,
                                 [Bikst, Bconst], [Bps[pb]])
                            K.cp("dve", ikt[:], ps[pb][:, 0:96], [Bps[pb]], [Bik])
                            K.op("dve", lambda e: e.bn_stats(out=ikstats[:], in_=ikt[:, 0:64]), [Bik], [Bik])
                            K.op("dve", lambda e: e.bn_aggr(out=ist[:, 0:2], in_=ikstats[:]), [Bik], [Bik])
                            K.act(ist[:, 2:3], ist[:, 1:2], AF.Sqrt, [Bik], [Bik], scale=1.0, bias=EPS)
                            K.op("dve", lambda e: e.reciprocal(out=ist[:, 4:5], in_=ist[:, 2:3]), [Bik], [Bik])
                            K.stt("dve", ist[:, 5:6], ist[:, 0:1], -1.0, ist[:, 4:5], ALU.mult, ALU.mult, [Bik], [Bik])
                            K.ts("dve", ikn[:], ikt[:, 0:64], ist[:, 4:5], ist[:, 5:6], ALU.mult, ALU.add, [Bik], [Bik])
                            K.tt("dve", ikn[:], ikn[:], ikgb[:, 0:64], ALU.mult, [Bik, Bconst], [Bik])
                            K.tt("dve", iknb[:], ikn[:], ikgb[:, 64:128], ALU.add, [Bik, Bconst], [Bik])
                            K.ts("dve", iwt[:, tb, :], ikt[:, 64:96], 32.0 ** -0.5, None, ALU.mult, None, [Bik], [Biw])
                            pb2 = nextx()
                            K.mm(ps[pb2][0:64, 0:128], iknb[:], identb[:], True, True, [Bik, Bconst], [Bps[pb2]])
                            K.cp("act", ikTs[:, tb * 128:(tb + 1) * 128], ps[pb2][0:64, 0:128], [Bps[pb2]], [BikTs])
                        K.dma("sp", ikT_d[:, g * 1024:(g + 1) * 1024], ikTs[:], r=[BikTs])
                        K.dma("sp", iw_d.rearrange("(kb p) c -> p kb c", p=128)[:, g * 8:(g + 1) * 8, :], iwt[:], r=[Biw])
                    return deferred

                tiles = []
                for h in range(16):
                    tiles.append((C_BK + h * 128, 128, epi_kT(kTb_d[h], 1.0, (0, 1), g * 1024)))
                for h in range(4):
                    tiles.append((C_AK + h * 128, 128, epi_kT(kTa_d[h], 1.0, (0, 1), g * 1024)))
                vb3 = vb_d.rearrange("(kb p) c -> p kb c", p=128)
                va3 = va_d.rearrange("(kb p) c -> p kb c", p=128)
                for h in range(16):
                    tiles.append((C_BV + h * 128, 128, epi_v(vb3, h * 128)))
                for h in range(4):
                    tiles.append((C_AV + h * 128, 128, epi_v(va3, h * 128)))
                tiles.append((C_IK, 96, epi_ik))
                hooks = {}
                if g + 1 < 8:
                    for tb in range(8):
                        hooks[4 * tb + 2] = (lambda g=g, tb=tb: emit_B1a(g + 1, tb))
                        hooks[4 * tb + 5] = (lambda g=g, tb=tb: emit_B1b(g + 1, tb))
                pj.run(W3in, tiles, uT, movbufs, (0, 1), hooks=hooks)
                if g % 2 == 1:
                    k = g // 2
                    qt = []
                    for h in range(16):
                        qt.append((C_AQ + h * 128, 128, epi_kT(qTa_d[h], 128.0 ** -0.5, (1,), k * 512)))
                    for h in range(16):
                        qt.append((C_BQ + h * 128, 128, epi_kT(qTb_d[h], 128.0 ** -0.5, (1,), k * 512)))
                    for h in range(16):
                        qt.append((C_IQ + h * 128, 128, epi_kT(iqT_d[h], 0.125, (1,), k * 512)))
                    pj.run(W3in, qt, uT, movbufs, (1,))
            K.barrier()

    if stop_after >= "C":
        with ExitStack() as ph:
            ikT2 = sb(ph, [128, S], BF16)
            kvrow = sb(ph, [1, S], F32)
            cneg = sb(ph, [128, 4, 512], BF16)
            cnegf = sb(ph, [128, 512], F32)
            Bk = Buf()
            K.dma("sp", ikT2[0:64, :], ikT_d[:, :], w=[Bk])
            K.dma("sp", ikT2[64:128, :], ikT_d[:, :], w=[Bk])
            K.dma("sp", kvrow[:], kvrow_d[:, :], w=[Bk])
            K.ts("dve", kvrow[:], kvrow[:], -1.0, BIG, ALU.add, ALU.mult, [Bk], [Bk])
            for r_ in range(4):
                K.dma("sp", cnegf[:], cneg_d[r_], w=[Bk])
                K.cp("dve", cneg[:, r_, :], cnegf[:], [Bk], [Bk])
            score = [sb(ph, [128, S], F32) for _ in range(2)]
            Bscore = bufs(2)
            maskb = sb(ph, [128, S], BF16)
            junk = maskb
            Bmask = Buf()
            diags = [sb(ph, [128, 32, 128], BF16) for _ in range(2)]
            Bdiags = bufs(2)
            iqs = Ring([sb(ph, [128, 16, 128], BF16) for _ in range(2)])
            iws = Ring([sb(ph, [128, 32], F32) for _ in range(2)])
            rb = Ring([sb(ph, [128, 1024], BF16) for _ in range(3)])
            mts = Ring([sb(ph, [128, 4, 128], BF16) for _ in range(3)])
            lo = sb(ph, [128, 4], F32)
            Blo = Buf()
            zi = [0]
            iw3 = iw_d.rearrange("(kb p) c -> p kb c", p=128)
            pend_tr = [None]

            def emit_transposes(k, r_, nk):
                for kb4 in range(nk):
                    pb = 6 + (kb4 % 2)
                    for i in range(4):
                        kb = kb4 * 4 + i
                        K.mm(ps[pb][:, i * 128:(i + 1) * 128], maskb[:, kb * 128:(kb + 1) * 128], identb[:], True, True,
                             [Bmask, Bconst], [Bps[pb]])
                    mt, Bmt = mts.next()
                    K.cp("dve", mt[:].rearrange("p a b -> p (a b)"), ps[pb][:, :], [Bps[pb]], [Bmt])
                    K.dma("sp", maskT_d[k, kb4 * 4:(kb4 + 1) * 4, :, r_ * 128:(r_ + 1) * 128].rearrange("a p c -> p a c"),
                          mt[:], r=[Bmt])

            qstate = {}

            def prefetch_q(qb):
                if qb >= 16 or qb in qstate:
                    return
                k, r_ = qb // 4, qb % 4
                pblk = (4 * k + 3) * 4 + r_
                iq_, Biq = iqs.next()
                iw_, Biw_ = iws.next()
                K.dma("sp", iq_[:], iqT_d[:, :, qb * 128:(qb + 1) * 128].rearrange("t p c -> p t c"), w=[Biq])
                K.dma("sp", iw_[:], iw3[:, pblk, :], w=[Biw_])
                diag, Bdiag = diags[qb % 2], Bdiags[qb % 2]
                for h in range(32):
                    K.ts("pool", diag[:, h, :], identb[:], iw_[:, h:h + 1], None, ALU.mult, None,
                         [Bconst, Biw_], [Bdiag])
                qstate[qb] = (iq_, Biq, diag, Bdiag)

            prefetch_q(0)
            for qb in range(16):
                k, r_ = qb // 4, qb % 4
                nk = 4 * (k + 1)
                nkeys = nk * 512
                iq_, Biq, diag, Bdiag = qstate[qb]
                prefetch_q(qb + 1)
                sc, Bsc = score[qb % 2], Bscore[qb % 2]
                for kc in range(nk):
                    sp_ = 4 + (kc % 2)
                    pend = None
                    for hp2 in range(16):
                        zj = zi[0] % 2
                        zi[0] += 1
                        for sub in (0, 1):
                            hp = 64 * sub
                            K.mm(ps[2 * zj + sub][:, :], iq_[hp:hp + 64, hp2, :], ikT2[hp:hp + 64, kc * 512:(kc + 1) * 512], True, True,
                                 [Biq, Bk], [Bps[2 * zj + sub]])
                        if pend is not None:
                            pend()
                        rt, Br = rb.next()
                        K.act(rt[:], psb[zj][:, :], AF.Relu, [Bps[2 * zj], Bps[2 * zj + 1]], [Br])

                        def pend(hp2=hp2, rt=rt, Br=Br, sp_=sp_, diag=diag, Bdiag=Bdiag):
                            for sub in (0, 1):
                                h = 2 * hp2 + sub
                                K.mm(ps[sp_][:, :], diag[:, h, :], rt[:, sub * 512:(sub + 1) * 512], h == 0, False, [Bdiag, Br],
                                     [Bps[sp_]])
                    pend()
                    last = (kc == nk - 1)
                    K.mm(ps[sp_][:, :], onesf[0:1, :], kvrow[0:1, kc * 512:(kc + 1) * 512], False, not last, [Bconst, Bk],
                         [Bps[sp_]])
                    if last:
                        K.mm(ps[sp_][:, :], identb[:], cneg[:, r_, :], False, True, [Bconst, Bk], [Bps[sp_]])
                    K.cp("act", sc[:, kc * 512:(kc + 1) * 512], ps[sp_][:, :], [Bps[sp_]], [Bsc])
                if pend_tr[0] is not None:
                    emit_transposes(*pend_tr[0])
                K.op("dve", lambda e: e.memset(lo[:, 0:1], -64.0), w=[Blo])
                step = 64.0
                for it in range(NBIS):
                    K.ts("dve", lo[:, 1:2], lo[:, 0:1], step, None, ALU.add, None, [Blo], [Blo])
                    K.ts("dve", junk[:, 0:nkeys], sc[:, 0:nkeys], lo[:, 1:2], 0.0, ALU.is_ge, ALU.add, [Bsc, Blo], [Blo, Bmask],
                         accum=lo[:, 2:3])
                    K.ts("dve", lo[:, 3:4], lo[:, 2:3], 255.5, step, ALU.is_ge, ALU.mult, [Blo], [Blo])
                    K.tt("dve", lo[:, 0:1], lo[:, 0:1], lo[:, 3:4], ALU.add, [Blo], [Blo])
                    step *= 0.5
                K.ts("dve", maskb[:, 0:nkeys], sc[:, 0:nkeys], lo[:, 0:1], None, ALU.is_ge, None, [Bsc, Blo], [Bmask])
                pend_tr[0] = (k, r_, nk)
            emit_transposes(*pend_tr[0])
            K.barrier()

    def run_pipeline(stages, items, reverse=False):
        n = len(items)
        ns = len(stages)
        for step in range(n + ns - 1):
            order = list(enumerate(stages))
            if reverse:
                order = order[::-1]
            for si, f in order:
                i = step - si
                if f is not None and 0 <= i < n:
                    f(items[i])

    if stop_after >= "D":
        with ExitStack() as ph:
            kT = Ring([sb(ph, [128, S], BF16) for _ in range(2)])
            vv = Ring([sb(ph, [128, 64, 128], BF16) for _ in range(2)])
            qq = Ring([sb(ph, [128, 4, 512], BF16) for _ in range(2)])
            mk = Ring([sb(ph, [128, 4, 512], BF16) for _ in range(6)])
            btr = Ring([sb(ph, [128, 2, 512], F32) for _ in range(6)])
            tmpr = Ring([sb(ph, [128, 1024], F32) for _ in range(3)])
            er = Ring([sb(ph, [128, 1024], BF16) for _ in range(6)])
            pr = Ring([sb(ph, [128, 1024], BF16) for _ in range(8)])
            rden = sb(ph, [128, 512], F32)
            Brden = Buf()
            osb = Ring([sb(ph, [128, 512], F32) for _ in range(2)])
            rb31 = sb(ph, [128, 16], F32)
            K.dma("sp", rb31[:], rb31_d[:, :], w=[Bconst])
            sqr_ = Ring([sb(ph, [128, 512], F32) for _ in range(2)])
            rowr = Ring([sb(ph, [1, 512], F32) for _ in range(2)])
            va3 = va_d.rearrange("(kb p) c -> p kb c", p=128)
            groups = [(k, g) for k in range(4) for g in range(4)]
            gctx = {}

            def load_group(gi):
                if gi >= len(groups) or gi in gctx:
                    return
                k, g = groups[gi]
                nkb = 16 * (k + 1)
                kt, Bkt = kT.next()
                vt, Bvt = vv.next()
                qt, Bqt = qq.next()
                K.dma("sp", kt[:, 0:nkb * 128], kTa_d[g][:, 0:nkb * 128], w=[Bkt])
                K.dma("sp", vt[:, 0:nkb, :], va3[:, 0:nkb, g * 128:(g + 1) * 128], w=[Bvt])
                K.dma("sp", qt[:], qTa_d[4 * g:4 * g + 4, :, k * 512:(k + 1) * 512].rearrange("h p c -> p h c"), w=[Bqt])
                gctx[gi] = (kt, Bkt, vt, Bvt, qt, Bqt)

            items = []
            hcount = 0
            for gi, (k, g) in enumerate(groups):
                nkb = 16 * (k + 1)
                for hh in range(4):
                    for pp in range(nkb // 2):
                        items.append(dict(gi=gi, k=k, g=g, hh=hh, pp=pp, kb0=2 * pp, nkb=nkb, hpar=hcount % 2, idx=len(items)))
                    hcount += 1
            li = [0]
            mstate = {}

            def S1(it):
                gi, k, g, hh, pp, kb0, nkb = it["gi"], it["k"], it["g"], it["hh"], it["pp"], it["kb0"], it["nkb"]
                if hh == 0 and pp == 0:
                    load_group(gi)
                if hh == 0 and pp == 6:
                    load_group(gi + 1)
                kt, Bkt, vt, Bvt, qt, Bqt = gctx[gi]
                h = 4 * g + hh
                for j_ in range(it["idx"], min(it["idx"] + 5, len(items))):
                    it2 = items[j_]
                    if "mk" not in it2:
                        if it2["kb0"] % 4 == 0:
                            mkt2, Bmk2 = mk.next()
                            K.dma("sp", mkt2[:], maskT_d[it2["k"], it2["kb0"]:it2["kb0"] + 4, :, :].rearrange("a p c -> p a c"),
                                  w=[Bmk2])
                            it2["mk"] = (mkt2, Bmk2)
                        else:
                            it2["mk"] = items[j_ - 1]["mk"]
                near = kb0 >= nkb - 6
                for j_ in range(it["idx"], min(it["idx"] + 4, len(items))):
                    it2 = items[j_]
                    if it2["kb0"] >= it2["nkb"] - 6 and "bt" not in it2:
                        bt2, Bbt2 = btr.next()
                        r6 = it2["kb0"] - (it2["nkb"] - 6)
                        K.dma("sp", bt2[:], Bt_d[4 * it2["g"] + it2["hh"], r6:r6 + 2].rearrange("a p c -> p a c"), w=[Bbt2])
                        it2["bt"] = (bt2, Bbt2)
                if near:
                    bt, Bbt = it["bt"]
                zj = li[0] % 2
                li[0] += 1
                for sub in (0, 1):
                    kb = kb0 + sub
                    K.mm(ps[2 * zj + sub][:, :], kt[:, kb * 128:(kb + 1) * 128], qt[:, hh, :], True, True, [Bkt, Bqt],
                         [Bps[2 * zj + sub]])
                et, Be = er.next()
                if near:
                    tm, Btm = tmpr.next()
                    K.tt("dve", tm[:], psb[zj][:, :], bt[:].rearrange("p a b -> p (a b)"), ALU.add,
                         [Bps[2 * zj], Bps[2 * zj + 1], Bbt], [Btm])
                    K.act(et[:], tm[:], AF.Exp, [Btm], [Be])
                else:
                    K.act(et[:], psb[zj][:, :], AF.Exp, [Bps[2 * zj], Bps[2 * zj + 1], Bconst], [Be], scale=1.0,
                          bias=rb31[:, h:h + 1])
                it["e"] = (et, Be)

            def S2(it):
                et, Be = it["e"]
                mkt, Bmk = it["mk"]
                kb0 = it["kb0"]
                pt, Bp = pr.next()
                m0 = kb0 % 4
                K.tt("pool" if it["pp"] % 4 == 0 else "dve", pt[:], et[:], mkt[:, m0:m0 + 2, :].rearrange("p a b -> p (a b)"),
                     ALU.mult, [Be, Bmk], [Bp])
                it["p"] = (pt, Bp)

            def S3(it):
                gi, k, g, hh, pp, kb0, nkb = it["gi"], it["k"], it["g"], it["hh"], it["pp"], it["kb0"], it["nkb"]
                kt, Bkt, vt, Bvt, qt, Bqt = gctx[gi]
                pt, Bp = it["p"]
                psN, psD = (4, 5) if it["hpar"] == 0 else (6, 7)
                lastp = (pp == nkb // 2 - 1)
                for sub in (0, 1):
                    K.mm(ps[psN][:, :], vt[:, kb0 + sub, :], pt[:, sub * 512:(sub + 1) * 512], pp == 0 and sub == 0,
                         lastp and sub == 1, [Bvt, Bp], [Bps[psN]])
                    K.mm(ps[psD][:, :], onesb[:], pt[:, sub * 512:(sub + 1) * 512], pp == 0 and sub == 0,
                         lastp and sub == 1, [Bconst, Bp], [Bps[psD]])
                if lastp:
                    h = 4 * g + hh
                    K.op("dve", lambda e, psD=psD: e.reciprocal(out=rden[:], in_=ps[psD][:, :]), [Bps[psD]], [Brden])
                    ot, Bo = osb.next()
                    K.tt("dve", ot[:], ps[psN][:, :], rden[:], ALU.mult, [Bps[psN], Brden], [Bo])
                    K.dma("sp", oT_d[h][:, k * 512:(k + 1) * 512], ot[:], r=[Bo])
                    sq, Bsq = sqr_.next()
                    K.act(sq[:], ot[:], AF.Square, [Bo], [Bsq])
                    K.mm(ps[psD][0:1, :], onesf[:, 0:1], sq[:], True, True, [Bconst, Bsq], [Bps[psD]])
                    rw, Brw = rowr.next()
                    K.cp("act", rw[:], ps[psD][0:1, :], [Bps[psD]], [Brw])
                    K.dma("sp", ssq_d[h:h + 1, k * 512:(k + 1) * 512], rw[:], r=[Brw])

            run_pipeline([S1, S2, None, None, S3], items)
            K.barrier()

    if stop_after >= "E":
        with ExitStack() as ph:
            kT = Ring([sb(ph, [128, S], BF16) for _ in range(2)])
            vv = Ring([sb(ph, [128, 64, 128], BF16) for _ in range(2)])
            qq = Ring([sb(ph, [128, 512], BF16) for _ in range(2)])
            qnr = Ring([sb(ph, [128, 512], BF16) for _ in range(2)])
            cmr = sb(ph, [128, 4, 512], BF16)
            cmf = sb(ph, [128, 512], F32)
            tri = sb(ph, [128, 128], BF16)
            trif = sb(ph, [128, 128], F32)
            Bc2 = Buf()
            for r_ in range(4):
                K.dma("sp", cmf[:], cm_d[:, 3 - r_, :], w=[Bc2])
                K.cp("dve", cmr[:, r_, :], cmf[:], [Bc2], [Bc2])
            K.dma("sp", trif[:], tri_d[:, :], w=[Bc2])
            K.cp("dve", tri[:], trif[:], [Bc2], [Bc2])
            efr = Ring([sb(ph, [128, 1024], F32) for _ in range(2)])
            spr = Ring([sb(ph, [128, 1024], BF16) for _ in range(4)])
            sprw = Ring([sb(ph, [128, 1024], BF16) for _ in range(2)])
            ar = Ring([sb(ph, [128, 1024], BF16) for _ in range(4)])
            arw = Ring([sb(ph, [128, 1024], BF16) for _ in range(2)])
            Rr = Ring([sb(ph, [128, 512], BF16) for _ in range(5)])
            osb = Ring([sb(ph, [128, 512], F32) for _ in range(2)])
            sqr_ = Ring([sb(ph, [128, 512], F32) for _ in range(2)])
            rowr = Ring([sb(ph, [1, 512], F32) for _ in range(2)])
            vb3 = vb_d.rearrange("(kb p) c -> p kb c", p=128)
            heads = [(k, h) for k in range(4) for h in range(16)]
            hctx = {}

            def load_head(hi):
                if hi >= len(heads) or hi in hctx:
                    return
                k, h = heads[hi]
                nkb = 16 * (k + 1)
                kt, Bkt = kT.next()
                vt, Bvt = vv.next()
                qt, Bqt = qq.next()
                qn, Bqn = qnr.next()
                K.dma("sp", kt[:, 0:nkb * 128], kTb_d[h][:, 0:nkb * 128], w=[Bkt])
                K.dma("sp", vt[:, 0:nkb, :], vb3[:, 0:nkb, h * 128:(h + 1) * 128], w=[Bvt])
                K.dma("sp", qt[:], qTb_d[h][:, k * 512:(k + 1) * 512], w=[Bqt])
                K.ts("dve", qn[:], qt[:], -1.0, None, ALU.mult, None, [Bqt], [Bqn])
                hctx[hi] = (kt, Bkt, vt, Bvt, qt, Bqt, qn, Bqn)

            items = []
            for hi, (k, h) in enumerate(heads):
                nkb = 16 * (k + 1)
                npr = nkb // 2
                for p in range(npr):
                    items.append(dict(hi=hi, k=k, h=h, p=p, kb0=nkb - 1 - 2 * p, nkb=nkb, first=(p == 0), last=(p == npr - 1)))
            lti = [0]
            rstate = {}

            def S1(it):
                hi, kb0 = it["hi"], it["kb0"]
                if it["first"]:
                    load_head(hi)
                if it["p"] == 5:
                    load_head(hi + 1)
                kt, Bkt, vt, Bvt, qt, Bqt, qn, Bqn = hctx[hi]
                for sub in (0, 1):
                    kb = kb0 - sub
                    K.mm(ps[sub][:, :], kt[:, kb * 128:(kb + 1) * 128], qt[:], True, True, [Bkt, Bqt], [Bps[sub]])
                ef, Bef = efr.next()
                K.act(ef[:], psb[0][:, :], AF.Exp, [Bps[0], Bps[1]], [Bef])
                spt, Bsp = spr.next()
                if it["p"] < 2:
                    sw, Bsw = sprw.next()
                    K.act(sw[:], ef[:], AF.Ln, [Bef], [Bsw], scale=1.0, bias=1.0)
                    K.tt("pool", spt[:], sw[:], cmr[:, 2 * it["p"]:2 * it["p"] + 2, :].rearrange("p a b -> p (a b)"), ALU.mult,
                         [Bsw, Bc2], [Bsp])
                else:
                    K.act(spt[:], ef[:], AF.Ln, [Bef], [Bsp], scale=1.0, bias=1.0)
                it["sp"] = (spt, Bsp)
                it["Rprev"] = None if it["first"] else rstate["R"]
                if not it["last"]:
                    Rn, BRn = Rr.next()
                    if it["first"]:
                        K.tt("pool", Rn[:], spt[:, 0:512], spt[:, 512:1024], ALU.add, [Bsp], [BRn])
                    else:
                        Rp, BRp = rstate["R"]
                        K.tt("pool", Rn[:], Rp[:], spt[:, 0:512], ALU.add, [BRp, Bsp], [BRn])
                        K.tt("pool", Rn[:], Rn[:], spt[:, 512:1024], ALU.add, [BRn, Bsp], [BRn])
                    rstate["R"] = (Rn, BRn)

            def S2(it):
                hi, kb0 = it["hi"], it["kb0"]
                kt, Bkt, vt, Bvt, qt, Bqt, qn, Bqn = hctx[hi]
                spt, Bsp = it["sp"]
                first = it["first"]
                lj = 1 + (lti[0] % 2)
                lti[0] += 1
                it["lj"] = lj
                b0, b1 = 2 * lj, 2 * lj + 1
                K.mm(ps[b0][:, :], tri[:], spt[:, 0:512], True, False, [Bc2, Bsp], [Bps[b0]])
                if not first:
                    Rp, BRp = it["Rprev"]
                    K.mm(ps[b0][:, :], onesb[:], Rp[:], False, False, [Bconst, BRp], [Bps[b0]])
                K.mm(ps[b0][:, :], kt[:, kb0 * 128:(kb0 + 1) * 128], qn[:], False, True, [Bkt, Bqn], [Bps[b0]])
                K.mm(ps[b1][:, :], tri[:], spt[:, 512:1024], True, False, [Bc2, Bsp], [Bps[b1]])
                K.mm(ps[b1][:, :], onesb[:], spt[:, 0:512], False, False, [Bconst, Bsp], [Bps[b1]])
                if not first:
                    K.mm(ps[b1][:, :], onesb[:], Rp[:], False, False, [Bconst, BRp], [Bps[b1]])
                K.mm(ps[b1][:, :], kt[:, (kb0 - 1) * 128:kb0 * 128], qn[:], False, True, [Bkt, Bqn], [Bps[b1]])

            def S3(it):
                lj = it["lj"]
                at, Ba = ar.next()
                if it["p"] < 2:
                    aw, Baw = arw.next()
                    K.act(aw[:], psb[lj][:, :], AF.Exp, [Bps[2 * lj], Bps[2 * lj + 1]], [Baw], scale=-1.0)
                    K.tt("pool", at[:], aw[:], cmr[:, 2 * it["p"]:2 * it["p"] + 2, :].rearrange("p a b -> p (a b)"), ALU.mult,
                         [Baw, Bc2], [Ba])
                else:
                    K.act(at[:], psb[lj][:, :], AF.Exp, [Bps[2 * lj], Bps[2 * lj + 1]], [Ba], scale=-1.0)
                it["a"] = (at, Ba)

            def S4(it):
                hi, k, h, kb0 = it["hi"], it["k"], it["h"], it["kb0"]
                kt, Bkt, vt, Bvt, qt, Bqt, qn, Bqn = hctx[hi]
                at, Ba = it["a"]
                psO = 6 + (hi % 2)
                K.mm(ps[psO][:, :], vt[:, kb0, :], at[:, 0:512], it["first"], False, [Bvt, Ba], [Bps[psO]])
                K.mm(ps[psO][:, :], vt[:, kb0 - 1, :], at[:, 512:1024], False, it["last"], [Bvt, Ba], [Bps[psO]])
                if it["last"]:
                    ot, Bo = osb.next()
                    K.cp("dve", ot[:], ps[psO][:, :], [Bps[psO]], [Bo])
                    K.dma("sp", oT_d[16 + h][:, k * 512:(k + 1) * 512], ot[:], r=[Bo])
                    sq, Bsq = sqr_.next()
                    K.tt("dve", sq[:], ot[:], ot[:], ALU.mult, [Bo], [Bsq])
                    K.mm(ps[psO][0:1, :], onesf[:, 0:1], sq[:], True, True, [Bconst, Bsq], [Bps[psO]])
                    rw, Brw = rowr.next()
                    K.cp("dve", rw[:], ps[psO][0:1, :], [Bps[psO]], [Brw])
                    K.dma("sp", ssq_d[16 + h:17 + h, k * 512:(k + 1) * 512], rw[:], r=[Brw])

            run_pipeline([S1, S2, S3, S4], items)
            K.barrier()

    if stop_after >= "F":
        with ExitStack() as ph:
            onT = sb(ph, [128, 32, 1024], BF16)
            BonT = bufs(32)
            otr = Ring([sb(ph, [128, 1024], F32) for _ in range(3)])
            ssqt = sb(ph, [16, 2, 1024], F32)
            Bssq = Buf()
            rbc = [sb(ph, [128, 1024], F32) for _ in range(2)]
            Brbc = bufs(2)
            stgf = Ring([sb(ph, [128, 1024], F32) for _ in range(3)])
            pj = Proj(ph)
            W3o = w3(w_out)
            for g in range(2):
                cols = slice(g * 1024, (g + 1) * 1024)
                for ab in (0, 1):
                    K.dma("sp", ssqt[0:16, ab, :], ssq_d[16 * ab:16 * ab + 16, cols], w=[Bssq])
                for ab in (0, 1):
                    for h in (0, 1):
                        pb = 2 * ab + h
                        K.mm(ps[pb][:, :], onesf[0:16, :], ssqt[0:16, ab, h * 512:(h + 1) * 512], True, True,
                             [Bconst, Bssq], [Bps[pb]])
                for ab in (0, 1):
                    for h in (0, 1):
                        pb = 2 * ab + h
                        K.act(rbc[ab][:, h * 512:(h + 1) * 512], ps[pb][:, :], AF.Sqrt, [Bps[pb]], [Brbc[ab]],
                              scale=1.0 / 2048.0, bias=EPS)
                    K.op("dve", lambda e, ab=ab: e.reciprocal(out=rbc[ab][:], in_=rbc[ab][:]), [Brbc[ab]], [Brbc[ab]])
                for j in range(32):
                    ot, Bo = otr.next()
                    K.dma("sp", ot[:], oT_d[j][:, cols], w=[Bo])
                    K.stt("dve", onT[:, j, :], ot[:], prmT[:, 4, j:j + 1], rbc[j // 16][:], ALU.mult,
                          ALU.mult, [Bo, Bconst, Brbc[j // 16]], [BonT[j]])

                def epi_mix(n_off, g=g):
                    def epi(pss, Bpss, idx):
                        s_, Bs = stgf.next()
                        n = idx
                        for h in (0, 1):
                            if (idx + h) % 2 == 0:
                                K.act(s_[:, h * 512:(h + 1) * 512], pss[h][:, :], AF.Identity, [Bpss[h], Bmod], [Bs],
                                      scale=modT[:, 64 + n:65 + n])
                            else:
                                K.ts("dve", s_[:, h * 512:(h + 1) * 512], pss[h][:, :], modT[:, 64 + n:65 + n], None,
                                     ALU.mult, None, [Bpss[h], Bmod], [Bs])
                        K.dma("sp", mixT_d[g * 8:(g + 1) * 8, :, n, :].rearrange("b p t -> p b t"),
                              s_[:].rearrange("p (b t) -> p b t", b=8), r=[Bs])
                        return None
                    return epi

                tiles = [(n * 128, 128, epi_mix(n)) for n in range(32)]
                pj.run(W3o, tiles, onT, lambda kc, h: [BonT[kc]], (0, 1), npairs=4)
            K.barrier()

    def res_ln_pass(ph, final):
        npart = 4 if final else 1
        part = [Ring([sb(ph, [128, 16, 128], F32) for _ in range(2 if final else 4)]) for _ in range(npart)]
        xr = Ring([sb(ph, [128, D], F32) for _ in range(2)])
        hpre = Ring([sb(ph, [128, D], F32) for _ in range(2)])
        gbc = sb(ph, [128, D], F32)
        bbc = sb(ph, [128, D], F32)
        Bgb = Buf()
        stats = sb(ph, [128, 48], F32)
        st = sb(ph, [128, 8], F32)
        Bst = Buf()
        stats2 = sb(ph, [128, 48], F32)
        st2 = sb(ph, [128, 8], F32)
        Bst2 = Buf()
        if not final:
            g0 = sb(ph, [128, D], F32)
            b0 = sb(ph, [128, D], F32)
            K.dma("sp", g0[:], bc_d[0], w=[Bgb])
            K.dma("sp", b0[:], bc_d[1], w=[Bgb])
            K.dma("sp", gbc[:], bc_d[2], w=[Bgb])
            K.dma("sp", bbc[:], bc_d[3], w=[Bgb])
            xhb = Ring([sb(ph, [128, D], BF16) for _ in range(2)])
            u2s = Ring([sb(ph, [128, 32, 128], BF16) for _ in range(2)])
        else:
            K.dma("sp", gbc[:], bc_d[4], w=[Bgb])
            K.dma("sp", bbc[:], bc_d[5], w=[Bgb])
        state = {}

        def stage_A(tb):
            k, r_ = tb // 4, tb % 4
            tcols = slice(tb * 128, (tb + 1) * 128)
            halves = []
            for hf in range(2):
                pts = []
                for q in range(npart):
                    pt, Bpt = part[q].next()
                    src = (yT_d[q, tb, :, hf * 16:(hf + 1) * 16, :] if final else
                           mixT_d[tb, :, hf * 16:(hf + 1) * 16, :])
                    K.dma("sp", pt[:], src, w=[Bpt])
                    pts.append((pt, Bpt))
                halves.append(pts)
                if hf == 0:
                    xt, Bx = xr.next()
                    if final:
                        K.dma("sp", xt[:], h1_d[tb * 128:(tb + 1) * 128, :], w=[Bx])
                    else:
                        r0 = (4 * k + 3) * 512 + r_ * 128
                        K.dma("sp", xt[:], xpad[r0:r0 + 128, :], w=[Bx])
            if not final:
                ln_stats(xt, Bx, stats, st, Bst)
                K.act(xt[:], xt[:], AF.Identity, [Bx, Bst], [Bx], scale=st[:, 4:5], bias=st[:, 5:6])
                K.tt("dve", xt[:], xt[:], g0[:], ALU.mult, [Bx, Bgb], [Bx])
                K.tt("pool", xt[:], xt[:], b0[:], ALU.add, [Bx, Bgb], [Bx])
            state[tb] = (halves, xt, Bx)

        def stage_B(tb):
            tcols = slice(tb * 128, (tb + 1) * 128)
            halves, resid, Bres = state.pop(tb)
            hp, Bhp = hpre.next()
            for n4 in range(8):
                pb = n4
                pts = halves[n4 // 4]
                for i in range(4):
                    n = (n4 % 4) * 4 + i
                    for q in range(npart):
                        K.mm(ps[pb][:, i * 128:(i + 1) * 128], pts[q][0][:, n, :], identf[:], q == 0, q == npart - 1,
                             [pts[q][1], Bconst], [Bps[pb]])
                K.stt("dve", hp[:, n4 * 512:(n4 + 1) * 512], resid[:, n4 * 512:(n4 + 1) * 512], ALPHA, ps[pb][:, :],
                      ALU.mult, ALU.add, [Bres, Bps[pb]], [Bhp])
            ln_stats(hp, Bhp, stats2, st2, Bst2)
            ot, Bo = hp, Bhp
            if not final:
                xb, Bxb = xhb.next()
                K.act(xb[:], hp[:], AF.Identity, [Bhp, Bst2], [Bxb], scale=st2[:, 4:5], bias=st2[:, 5:6])
            K.act(ot[:], hp[:], AF.Identity, [Bhp, Bst2], [Bo], scale=st2[:, 4:5], bias=st2[:, 5:6])
            K.tt("dve", ot[:], ot[:], gbc[:], ALU.mult, [Bo, Bgb], [Bo])
            K.tt("pool", ot[:], ot[:], bbc[:], ALU.add, [Bo, Bgb], [Bo])
            if final:
                K.dma("sp", out_d[tb * 128:(tb + 1) * 128, :], ot[:], r=[Bo])
            else:
                K.dma("sp", h1_d[tb * 128:(tb + 1) * 128, :], ot[:], r=[Bo])
                u2, Bu2 = u2s.next()
                to_featmajor(xb, Bxb, AB[:, 2, :], AB[:, 3, :], lambda kc, u2=u2: u2[:, kc, :], lambda kc, Bu2=Bu2: Bu2,
                             [0, 1, 2, 3])
                K.dma("sp", u2T_d[:, :, tcols], u2[:], r=[Bu2])

        if final:
            for tb in range(16):
                stage_A(tb)
                stage_B(tb)
        else:
            run_pipeline([stage_A, stage_B], list(range(16)))

    if stop_after >= "F":
        with ExitStack() as ph:
            res_ln_pass(ph, False)
            K.barrier()

    if stop_after >= "G":
        with ExitStack() as ph:
            u2T = sb(ph, [128, 32, 1024], BF16)
            Bu2T = Buf()
            hT = sb(ph, [128, 32, 1024], BF16)
            BhT = bufs(32)
            rl = Ring([sb(ph, [128, 512], F32) for _ in range(4)])
            stgf = Ring([sb(ph, [128, 1024], F32) for _ in range(2)])
            pj = Proj(ph)
            for g in range(2):
                K.dma("sp", u2T[:], u2T_d[:, :, g * 1024:(g + 1) * 1024], w=[Bu2T])
                for q in range(4):
                    def epi_up(pss, Bpss, idx):
                        for h in (0, 1):
                            rt, Br = rl.next()
                            K.act(rt[:], pss[h][:, :], AF.Relu, [Bpss[h]], [Br])
                            K.tt("pool" if h == 0 else "dve", hT[:, idx, h * 512:(h + 1) * 512], rt[:], rt[:], ALU.mult, [Br],
                                 [BhT[idx]])
                        return None

                    def epi_dn(q=q, g=g):
                        def epi(pss, Bpss, idx):
                            s_, Bs = stgf.next()
                            n = idx
                            for h in (0, 1):
                                if (idx + h) % 2 == 0:
                                    K.act(s_[:, h * 512:(h + 1) * 512], pss[h][:, :], AF.Identity, [Bpss[h], Bmod], [Bs],
                                          scale=modT[:, 160 + n:161 + n])
                                else:
                                    K.ts("dve", s_[:, h * 512:(h + 1) * 512], pss[h][:, :], modT[:, 160 + n:161 + n], None,
                                         ALU.mult, None, [Bpss[h], Bmod], [Bs])
                            K.dma("sp", yT_d[q, g * 8:(g + 1) * 8, :, n, :].rearrange("b p t -> p b t"),
                                  s_[:].rearrange("p (b t) -> p b t", b=8), r=[Bs])
                            return None
                        return epi

                    W3u = w3(w_up)
                    tiles = [(q * 4096 + n * 128, 128, epi_up) for n in range(32)]
                    pj.run(W3u, tiles, u2T, lambda kc, h: [Bu2T], (0, 1), npairs=4)
                    W3d = w3(w_down, q * 4096)
                    e_dn = epi_dn()
                    tiles = [(n * 128, 128, e_dn) for n in range(32)]
                    pj.run(W3d, tiles, hT, lambda kc, h: [BhT[kc]], (0, 1), npairs=4)
            K.barrier()
        with ExitStack() as ph:
            res_ln_pass(ph, True)
            K.barrier()

    K.barrier()
    K.finish()
    G.close()
    return nc, K


def _t5_bucket(n):
    n = np.maximum(n, 0)
    nf = np.maximum(n, 1).astype(np.float32)
    large = 16 + (np.log(nf / np.float32(16)) / np.float32(math.log(128 / 16)) * np.float32(16)).astype(np.int32)
    large = np.minimum(large, 31)
    return np.where(n < 16, n, large)


def _consts():
    identf = np.eye(128, dtype=np.float32)
    j = np.arange(128)[:, None]
    s = np.arange(128)[None, :]
    tri = (j >= s).astype(np.float32)
    t = np.arange(128)[:, None]
    sl = np.arange(512)[None, :]
    cneg = np.stack([np.where(sl > r * 128 + t, -BIG, 0.0) for r in range(4)]).astype(np.float32)
    sk = np.arange(128)[:, None]
    tq = np.arange(512)[None, :]
    cm = np.stack([(r * 128 + sk < tq) for r in range(4)], axis=1).astype(np.float32)
    return identf, tri, cneg, np.ascontiguousarray(cm)


def _bias_tiles(rel_bias):
    sk = np.arange(128)[:, None]
    tq = np.arange(512)[None, :]
    out = np.empty((16, 6, 128, 512), np.float32)
    for r6 in range(6):
        r = r6 - 1
        dist = (1 - r) * 128 + tq - sk
        bk = _t5_bucket(dist.astype(np.int32))
        out[:, r6] = np.transpose(rel_bias[bk], (2, 0, 1))
    return out


def make_in_maps(x, c, in_ln_g, in_ln_b, rel_bias, w_ada, b_ada, w_in, idx_kn_g, idx_kn_b, gn_sparse_g, gn_sb_g,
                 w_out, ln1_g, ln1_b, w_up, w_down, ln2_g, ln2_b):
    f = lambda a: np.ascontiguousarray(np.asarray(a, dtype=np.float32))
    x = f(x)
    identf, tri, cneg, cm = _consts()
    T32 = lambda v: np.ascontiguousarray(f(v).reshape(-1, 128).T)
    prmT = np.ascontiguousarray(np.stack([T32(in_ln_g), T32(in_ln_b), T32(ln1_g[0]), T32(ln1_b[0]),
                                          T32(np.concatenate([f(gn_sparse_g[0]), f(gn_sb_g[0])]))], axis=1))
    bc = np.ascontiguousarray(np.stack([np.broadcast_to(f(v).reshape(1, D), (128, D)) for v in
                                        (in_ln_g, in_ln_b, ln1_g[0], ln1_b[0], ln2_g[0], ln2_b[0])]))
    ikgb = np.ascontiguousarray(np.concatenate([np.broadcast_to(f(idx_kn_g[0]).reshape(1, 64), (128, 64)),
                                                np.broadcast_to(f(idx_kn_b[0]).reshape(1, 64), (128, 64))], axis=1))
    rel_bias = f(rel_bias)
    rb31 = np.ascontiguousarray(np.broadcast_to(rel_bias[31].reshape(1, 16), (128, 16)))
    Bt = _bias_tiles(rel_bias)
    shared = dict(badaT=T32(b_ada[0]), prmT=prmT, bc=bc, ikgb=ikgb, rb31=rb31, Bt=Bt, identf=identf, tri=tri,
                  cneg=cneg, cm=cm, w_ada=f(w_ada[0]), w_in=f(w_in[0]), w_out=f(w_out[0]), w_up=f(w_up[0]),
                  w_down=f(w_down[0]))
    maps = []
    for core in range(8):
        b, j = core // 4, core % 4
        xp = np.zeros((S, D), np.float32)
        kval = np.zeros((S,), np.float32)
        for p in range(16):
            cch = p - 3 + j
            if cch >= 0:
                xp[p * 512:(p + 1) * 512] = x[b, cch * 512:(cch + 1) * 512]
                kval[p * 512:(p + 1) * 512] = 1.0
        m = dict(shared)
        m.update(xpad=xp, kvalT=np.ascontiguousarray(kval.reshape(64, 128).T), kvrow=kval.reshape(1, S).copy(),
                 cT=T32(f(c)[b]))
        maps.append(m)
    return maps


_CACHE = {}


def kernel(**inputs):
    maps = make_in_maps(**inputs)
    if "nc" not in _CACHE:
        _CACHE["nc"] = build()[0]
    res = run_bass_kernel_spmd(_CACHE["nc"], maps, core_ids=list(range(8)))
    out = np.empty((2, S, D), np.float32)
    for core in range(8):
        b, j = core // 4, core % 4
        o = res.results[core]["out"]
        for k in range(4):
            cch = 4 * k + j
            out[b, cch * 512:(cch + 1) * 512] = o[k * 512:(k + 1) * 512]
    return out
```
